# Optimizing a Trainium2 kernel written in Bass

```python
import math
import jax, jax.numpy as jnp
from jax import lax
import numpy as np

D_MODEL = 1024
BATCH = 4
SEQ = 4096
DEPTH = 1

CHUNK = 64
RMS_EPS = 1e-6
RW_HEADS = 8
RW_HEAD_DIM = 64
RW_WIDTH = RW_HEADS * RW_HEAD_DIM
RW_DECAY_LORA = 64
RW_AAA_LORA = 64
RW_GATE_LORA = 128
RW_GN_EPS = 64e-5
ML_HEADS = 4
ML_HEAD_DIM = 128
ML_WIDTH = ML_HEADS * ML_HEAD_DIM
ML_CONV = 4
ML_NORM_EPS = 1e-5
N_BRANCH = 2
PEER_HEADS = 8
PEER_KEYS = 128
PEER_EXPERTS = PEER_KEYS * PEER_KEYS
PEER_QDIM = 256
PEER_HALF = PEER_QDIM // 2
PEER_TOPK = 16
PEER_TOKEN_BLOCK = 128
RW_COLS = 3 * RW_WIDTH + RW_DECAY_LORA + RW_AAA_LORA + RW_GATE_LORA
ML_COLS = 4 * ML_WIDTH + 2 * ML_HEADS
GATE_COLS = N_BRANCH * D_MODEL
IN_COLS = RW_COLS + ML_COLS + GATE_COLS

kernel_name = "hybrid_rwkv7_mlstm_peer_block"

F32 = jnp.float32


def _rmsnorm(x, g):
    xf = x.astype(F32)
    y = xf * lax.rsqrt(jnp.mean(xf * xf, axis=-1, keepdims=True) + RMS_EPS)
    return (y * g.astype(F32)).astype(x.dtype)


def _causal_conv(x, w, b):
    c = x.shape[-1]
    y = lax.conv_general_dilated(x, w[:, None, :], window_strides=(1,), padding=[(w.shape[0] - 1, 0)],
                                 dimension_numbers=('NWC', 'WIO', 'NWC'), feature_group_count=c)
    return y + b


def _rwkv7(z, mu, w0, w_up, a0, a_up, g_up, k_k, k_a, r_k, gn_w, gn_b):
    bsz, t_len, _ = z.shape
    z = z.astype(F32)
    z_prev = jnp.pad(z, ((0, 0), (1, 0), (0, 0)))[:, :-1]
    zs = z + (z_prev - z) * mu.astype(F32)
    o1 = RW_WIDTH; o2 = 2 * RW_WIDTH; o3 = 3 * RW_WIDTH
    o4 = o3 + RW_DECAY_LORA; o5 = o4 + RW_AAA_LORA
    r, k, v = zs[..., :o1], zs[..., o1:o2], zs[..., o2:o3]
    zw, za, zg = zs[..., o3:o4], zs[..., o4:o5], zs[..., o5:]
    w_log = -jax.nn.softplus(-(w0.astype(F32) + jnp.tanh(zw) @ w_up.astype(F32))) - 0.5
    decay = jnp.exp(-jnp.exp(w_log))
    a = jax.nn.sigmoid(a0.astype(F32) + za @ a_up.astype(F32))
    g = jax.nn.sigmoid(zg) @ g_up.astype(F32)
    hs = lambda t: t.reshape(bsz, t_len, RW_HEADS, RW_HEAD_DIM)
    r, k, v, decay, a = hs(r), hs(k), hs(v), hs(decay), hs(a)
    kk = k * k_k.astype(F32).reshape(RW_HEADS, RW_HEAD_DIM)
    kk = kk / jnp.maximum(jnp.sqrt(jnp.sum(kk * kk, axis=-1, keepdims=True)), 1e-12)
    k = k * (1.0 + (a - 1.0) * k_a.astype(F32).reshape(RW_HEADS, RW_HEAD_DIM))

    def step(S, inp):
        r_t, w_t, k_t, v_t, kk_t, b_t = inp
        sa = jnp.einsum('bhvk,bhk->bhv', S, -kk_t)
        S = S * w_t[:, :, None, :] + sa[..., None] * b_t[:, :, None, :] + v_t[..., None] * k_t[:, :, None, :]
        return S, jnp.einsum('bhvk,bhk->bhv', S, r_t)

    tm = lambda t: jnp.moveaxis(t, 1, 0)
    s0 = jnp.zeros((bsz, RW_HEADS, RW_HEAD_DIM, RW_HEAD_DIM), F32)
    _, y = lax.scan(step, s0, (tm(r), tm(decay), tm(k), tm(v), tm(kk), tm(kk * a)))
    y = jnp.moveaxis(y, 0, 1)
    mean = jnp.mean(y, axis=-1, keepdims=True)
    var = jnp.mean(jnp.square(y - mean), axis=-1, keepdims=True)
    y = (y - mean) * lax.rsqrt(var + RW_GN_EPS) * gn_w.astype(F32).reshape(RW_HEADS, RW_HEAD_DIM) \
        + gn_b.astype(F32).reshape(RW_HEADS, RW_HEAD_DIM)
    y = y + jnp.sum(r * k * r_k.astype(F32), axis=-1, keepdims=True) * v
    return y.reshape(bsz, t_len, RW_WIDTH) * g


def _mlstm(z, cq_w, cq_b, ck_w, ck_b, b_i, b_f, norm_w):
    bsz, t_len, _ = z.shape
    z = z.astype(F32)
    w1 = ML_WIDTH; w2 = 2 * ML_WIDTH; w3 = 3 * ML_WIDTH; w4 = 4 * ML_WIDTH
    q_pre, k_pre, v, o_pre = z[..., :w1], z[..., w1:w2], z[..., w2:w3], z[..., w3:w4]
    i_pre, f_pre = z[..., w4:w4 + ML_HEADS], z[..., w4 + ML_HEADS:]
    q = jax.nn.silu(_causal_conv(q_pre, cq_w.astype(F32), cq_b.astype(F32)))
    k = jax.nn.silu(_causal_conv(k_pre, ck_w.astype(F32), ck_b.astype(F32)))
    n_chunks = t_len // CHUNK
    chunked = lambda t: t.reshape(bsz, n_chunks, CHUNK, ML_HEADS, ML_HEAD_DIM).transpose(0, 3, 1, 2, 4)
    gate_c = lambda t: t.reshape(bsz, n_chunks, CHUNK, ML_HEADS).transpose(0, 3, 1, 2)
    q = chunked(q)
    k = chunked(k) * (ML_HEAD_DIM ** -0.5)
    v = chunked(v)
    ig = gate_c(i_pre + b_i.astype(F32))
    lf = jax.nn.log_sigmoid(gate_c(f_pre + b_f.astype(F32)))
    bcum = jnp.cumsum(lf, axis=-1)
    causal = jnp.tril(jnp.ones((CHUNK, CHUNK), dtype=bool))
    d_log = jnp.where(causal, bcum[..., :, None] - bcum[..., None, :] + ig[..., None, :], -jnp.inf)
    g_end = bcum[..., -1]
    a_s = g_end[..., None] - bcum + ig
    m_loc = jnp.max(a_s, axis=-1)
    wts = jnp.exp(a_s - m_loc[..., None])
    c_loc = jnp.einsum('bhcl,bhclv,bhclk->bhcvk', wts, v, k)
    n_loc = jnp.einsum('bhcl,bhclk->bhck', wts, k)

    def step(carry, inp):
        c_st, n_st, m_st = carry
        c_l, n_l, m_l, g_c = inp
        m_new = jnp.maximum(g_c + m_st, m_l)
        s_old = jnp.exp(g_c + m_st - m_new)
        s_new = jnp.exp(m_l - m_new)
        c_next = s_old[..., None, None] * c_st + s_new[..., None, None] * c_l
        n_next = s_old[..., None] * n_st + s_new[..., None] * n_l
        return (c_next, n_next, m_new), (c_st, n_st, m_st)

    init = (jnp.zeros((bsz, ML_HEADS, ML_HEAD_DIM, ML_HEAD_DIM), F32),
            jnp.zeros((bsz, ML_HEADS, ML_HEAD_DIM), F32),
            jnp.zeros((bsz, ML_HEADS), F32))
    mv = lambda t: jnp.moveaxis(t, 2, 0)
    _, (c_in, n_in, m_in) = lax.scan(step, init, (mv(c_loc), mv(n_loc), mv(m_loc), mv(g_end)))
    c_in = jnp.moveaxis(c_in, 0, 2)
    n_in = jnp.moveaxis(n_in, 0, 2)
    m_in = jnp.moveaxis(m_in, 0, 2)
    inter_log = bcum + m_in[..., None]
    m_t = jnp.maximum(jnp.max(d_log, axis=-1), inter_log)
    scores = jnp.einsum('bhcld,bhcsd->bhcls', q, k) * jnp.exp(d_log - m_t[..., None])
    inter_w = jnp.exp(inter_log - m_t)
    num = jnp.einsum('bhcls,bhcsd->bhcld', scores, v) + inter_w[..., None] * jnp.einsum('bhcvk,bhclk->bhclv', c_in, q)
    den = jnp.sum(scores, axis=-1) + inter_w * jnp.einsum('bhck,bhclk->bhcl', n_in, q)
    h = num / jnp.maximum(jnp.abs(den), jnp.exp(-m_t))[..., None]
    h = h.transpose(0, 2, 3, 1, 4).reshape(bsz, t_len, ML_HEADS, ML_HEAD_DIM)
    mean = jnp.mean(h, axis=-1, keepdims=True)
    var = jnp.mean(jnp.square(h - mean), axis=-1, keepdims=True)
    h = (h - mean) * lax.rsqrt(var + ML_NORM_EPS)
    h = h.reshape(bsz, t_len, ML_WIDTH) * norm_w.astype(F32)
    return h * jax.nn.sigmoid(o_pre)


def _peer(h, w_q, sub_keys, u_emb, v_emb):
    bsz, t_len, d = h.shape
    tok = h.reshape(-1, d)
    n_tok = tok.shape[0]
    q = (tok @ w_q).astype(F32).reshape(n_tok, PEER_HEADS, 2, PEER_HALF)
    s = jnp.einsum('nhpc,hpkc->nhpk', q, sub_keys.astype(F32))
    s_top, i_top = lax.top_k(s, PEER_TOPK)
    cand = (s_top[:, :, 0, :, None] + s_top[:, :, 1, None, :]).reshape(n_tok, PEER_HEADS, PEER_TOPK * PEER_TOPK)
    cand_idx = (i_top[:, :, 0, :, None] * PEER_KEYS + i_top[:, :, 1, None, :]).reshape(n_tok, PEER_HEADS, PEER_TOPK * PEER_TOPK)
    best, pos = lax.top_k(cand, PEER_TOPK)
    expert = jnp.take_along_axis(cand_idx, pos, axis=-1)
    gate = jax.nn.softmax(best, axis=-1)
    n_blk = n_tok // PEER_TOKEN_BLOCK
    sel = PEER_HEADS * PEER_TOPK

    def retrieve(args):
        tb, eb, gb = args
        act = jax.nn.gelu(jnp.einsum('tkd,td->tk', u_emb[eb], tb), approximate=False) * gb
        return jnp.einsum('tk,tkd->td', act, v_emb[eb])

    out = lax.map(retrieve, (tok.reshape(n_blk, PEER_TOKEN_BLOCK, d),
                             expert.reshape(n_blk, PEER_TOKEN_BLOCK, sel),
                             gate.astype(tok.dtype).reshape(n_blk, PEER_TOKEN_BLOCK, sel)))
    return out.reshape(bsz, t_len, d)


def setup_inputs(seed: int = 0) -> dict:
    key = jax.random.key(seed)
    ks = jax.random.split(key, 40)
    nrm = lambda k, shape, scale: jax.random.normal(k, shape, F32) * scale
    L = DEPTH
    return {
        "x": nrm(ks[0], (BATCH, SEQ, D_MODEL), 1.0),
        "norm1_g": 1.0 + nrm(ks[1], (L, D_MODEL), 0.02),
        "w_in": nrm(ks[2], (L, D_MODEL, IN_COLS), D_MODEL ** -0.5),
        "rw_mu": jax.random.uniform(ks[3], (L, RW_COLS), F32, 0.0, 1.0),
        "rw_w0": jnp.broadcast_to(jnp.linspace(-6.0, -1.0, RW_WIDTH, dtype=F32), (L, RW_WIDTH)) + nrm(ks[4], (L, RW_WIDTH), 0.1),
        "rw_w_up": nrm(ks[5], (L, RW_DECAY_LORA, RW_WIDTH), 0.1),
        "rw_a0": nrm(ks[6], (L, RW_WIDTH), 0.1),
        "rw_a_up": nrm(ks[7], (L, RW_AAA_LORA, RW_WIDTH), 0.1),
        "rw_g_up": nrm(ks[8], (L, RW_GATE_LORA, RW_WIDTH), RW_GATE_LORA ** -0.5),
        "rw_k_k": 0.85 + nrm(ks[9], (L, RW_WIDTH), 0.02),
        "rw_k_a": 1.0 + nrm(ks[10], (L, RW_WIDTH), 0.02),
        "rw_r_k": nrm(ks[11], (L, RW_HEADS, RW_HEAD_DIM), 0.1),
        "rw_gn_w": 1.0 + nrm(ks[12], (L, RW_WIDTH), 0.02),
        "rw_gn_b": nrm(ks[13], (L, RW_WIDTH), 0.02),
        "ml_conv_q_w": nrm(ks[14], (L, ML_CONV, ML_WIDTH), 0.5),
        "ml_conv_q_b": nrm(ks[15], (L, ML_WIDTH), 0.02),
        "ml_conv_k_w": nrm(ks[16], (L, ML_CONV, ML_WIDTH), 0.5),
        "ml_conv_k_b": nrm(ks[17], (L, ML_WIDTH), 0.02),
        "ml_b_i": nrm(ks[18], (L, ML_HEADS), 0.1),
        "ml_b_f": jnp.broadcast_to(jnp.linspace(3.0, 6.0, ML_HEADS, dtype=F32), (L, ML_HEADS)) + nrm(ks[19], (L, ML_HEADS), 0.1),
        "ml_norm_w": 1.0 + nrm(ks[20], (L, ML_WIDTH), 0.02),
        "gate_b": nrm(ks[21], (L, GATE_COLS), 0.02),
        "p_rw": nrm(ks[22], (L, RW_WIDTH, D_MODEL), RW_WIDTH ** -0.5),
        "p_ml": nrm(ks[23], (L, ML_WIDTH, D_MODEL), ML_WIDTH ** -0.5),
        "w_out": nrm(ks[24], (L, D_MODEL, D_MODEL), D_MODEL ** -0.5),
        "norm2_g": 1.0 + nrm(ks[25], (L, D_MODEL), 0.02),
        "peer_w_q": nrm(ks[26], (L, D_MODEL, PEER_HEADS * PEER_QDIM), D_MODEL ** -0.5),
        "peer_sub_keys": nrm(ks[27], (L, PEER_HEADS, 2, PEER_KEYS, PEER_HALF), PEER_HALF ** -0.5),
        "peer_u": nrm(ks[28], (L, PEER_EXPERTS, D_MODEL), D_MODEL ** -0.5),
        "peer_v": nrm(ks[29], (L, PEER_EXPERTS, D_MODEL), 0.25),
        "final_g": 1.0 + nrm(ks[30], (D_MODEL,), 0.02),
    }


def reference(x, norm1_g, w_in, rw_mu, rw_w0, rw_w_up, rw_a0, rw_a_up, rw_g_up, rw_k_k, rw_k_a, rw_r_k,
              rw_gn_w, rw_gn_b, ml_conv_q_w, ml_conv_q_b, ml_conv_k_w, ml_conv_k_b, ml_b_i, ml_b_f, ml_norm_w,
              gate_b, p_rw, p_ml, w_out, norm2_g, peer_w_q, peer_sub_keys, peer_u, peer_v, final_g):
    bsz, t_len, d = x.shape
    for l in range(DEPTH):
        h = _rmsnorm(x, norm1_g[l])
        z = h @ w_in[l]
        z_rw = z[..., :RW_COLS]
        z_ml = z[..., RW_COLS:RW_COLS + ML_COLS]
        z_gate = z[..., RW_COLS + ML_COLS:]
        y_rw = _rwkv7(z_rw, rw_mu[l], rw_w0[l], rw_w_up[l], rw_a0[l], rw_a_up[l], rw_g_up[l],
                      rw_k_k[l], rw_k_a[l], rw_r_k[l], rw_gn_w[l], rw_gn_b[l]).astype(x.dtype)
        y_ml = _mlstm(z_ml, ml_conv_q_w[l], ml_conv_q_b[l], ml_conv_k_w[l], ml_conv_k_b[l],
                      ml_b_i[l], ml_b_f[l], ml_norm_w[l]).astype(x.dtype)
        gate = jax.nn.sigmoid((z_gate + gate_b[l]).astype(F32)).astype(x.dtype).reshape(bsz, t_len, N_BRANCH, d)
        merged = gate[:, :, 0, :] * (y_rw @ p_rw[l]) + gate[:, :, 1, :] * (y_ml @ p_ml[l])
        x = x + merged @ w_out[l]
        x = x + _peer(_rmsnorm(x, norm2_g[l]), peer_w_q[l], peer_sub_keys[l], peer_u[l], peer_v[l])
    return _rmsnorm(x, final_g)
```

```python
import numpy as np
from contextlib import ExitStack
import concourse.bass as bass
import concourse.mybir as mybir
from concourse.bass_utils import run_bass_kernel_spmd

F32 = mybir.dt.float32
BF16 = mybir.dt.bfloat16
U32 = mybir.dt.uint32
I32 = mybir.dt.int32
AF = mybir.ActivationFunctionType
ALU = mybir.AluOpType
AX = mybir.AxisListType

ENGS = ["sync", "scalar", "vector", "gpsimd", "tensor"]

RW0, ML0, GT0 = 0, 1792, 3848


def _piece_cols():
    pcs = []
    ar = lambda a, n: list(range(a, a + n))
    for ab in "AB":
        for hp in range(4):
            pcs.append(("rw%s%d" % (ab, hp), ar(RW0 + hp * 128, 128) + ar(RW0 + 512 + hp * 128, 128) + ar(RW0 + 1024 + hp * 128, 128)))
    pcs.append(("lo", ar(RW0 + 1536, 256) + ar(RW0 + 1536, 256)))
    for h in range(4):
        pcs.append(("mlqk%d" % h, ar(ML0 + h * 128, 128) + ar(ML0 + 512 + h * 128, 128)))
    pcs.append(("mlv", ar(ML0 + 1024, 512)))
    pcs.append(("mlo", ar(ML0 + 1536, 512)))
    pcs.append(("mlif", ar(ML0 + 2048, 8)))
    for dc in range(8):
        pcs.append(("gate%d" % dc, ar(GT0 + dc * 128, 128) + ar(GT0 + 1024 + dc * 128, 128)))
    return pcs


PIECES = _piece_cols()
PIDX = {}
_off = 0
for _i, (_n, _c) in enumerate(PIECES):
    PIDX[_n] = (_i, _off, len(_c))
    _off += len(_c)
NWCOLS = _off
N_INPIECE = len(PIECES)
PI_PRW = N_INPIECE
PI_PML = N_INPIECE + 1
PI_WO0 = N_INPIECE + 2
PI_WO1 = N_INPIECE + 3
NPIECE = N_INPIECE + 4
NMU = 1536 * 2 + 512

PV = {}
_o = 0
for _n, _k in [("g1", 8), ("w0", 4), ("a0", 4), ("k_k", 4), ("k_a", 4), ("r_k", 4), ("gn_w", 4), ("gn_b", 4),
               ("cq_w", 16), ("cq_b", 4), ("ck_w", 16), ("ck_b", 4), ("gate_b", 16), ("g2", 8), ("b_i", 4),
               ("b_f", 4), ("flag", 1)]:
    PV[_n] = _o
    _o += _k
NPV = _o


STAGES = "ABCD"
CCUT = 99
NCH = 8
NHP = 4
CSUB = 99
DUMP = False
PTILES = list(range(16))
NGRP = 64
BLKS = list(range(8))


class Prog:
    NPOOL = 24

    def __init__(self, nc, stack):
        self.nc = nc
        self.esem = {e: stack.enter_context(nc.semaphore("s_" + e)) for e in ENGS}
        self.ecnt = {e: 0 for e in ENGS}
        self.pool = [stack.enter_context(nc.semaphore("d%d" % i)) for i in range(self.NPOOL)]
        self.pcnt = [0] * self.NPOOL
        self.pnext = 0
        self.sems = {}
        for e in ENGS:
            self.sems[("e", e)] = self.esem[e]
        for i in range(self.NPOOL):
            self.sems[("p", i)] = self.pool[i]
        self.seen = {e: {} for e in ENGS}
        self.last_w = {}
        self.readers = {}
        self.n_ops = 0
        self.n_waits = 0
        self.q = {e: [] for e in ENGS}
        self.children = {}
        self.alias = {}
        self.excl = set()

    def _exp(self, keys):
        out = []
        for k_ in keys:
            k_ = self.alias.get(k_, k_)
            out.append(k_)
            out.extend(self.children.get(k_, ()))
        return tuple(out)

    def _rw(self, reads, writes):
        reads = self._exp(reads)
        writes = list(self._exp(writes))
        r2 = []
        for k_ in reads:
            if k_ in self.excl:
                if k_ not in writes:
                    writes.append(k_)
            else:
                r2.append(k_)
        return tuple(r2), tuple(writes)

    def _deps(self, reads, writes):
        need = {}
        for k in reads:
            ev = self.last_w.get(k)
            if ev is not None:
                need[ev[0]] = max(need.get(ev[0], 0), ev[1])
        for k in writes:
            ev = self.last_w.get(k)
            if ev is not None:
                need[ev[0]] = max(need.get(ev[0], 0), ev[1])
            for ev in self.readers.get(k, ()):
                need[ev[0]] = max(need.get(ev[0], 0), ev[1])
        return need

    def _emit_waits(self, eng, need, skip_self=False):
        for sid, val in need.items():
            if skip_self and sid == ("e", eng):
                continue
            if self.seen[eng].get(sid, 0) >= val:
                continue
            self.seen[eng][sid] = val
            self.q[eng].append(("wait", sid, val))
            self.n_waits += 1

    def _record(self, ev, reads, writes):
        for k in writes:
            self.last_w[k] = ev
            self.readers[k] = []
        for k in reads:
            if k in writes:
                continue
            self.readers.setdefault(k, []).append(ev)

    def op(self, eng, fn, reads=(), writes=()):
        reads, writes = self._rw(reads, writes)
        need = self._deps(reads, writes)
        self._emit_waits(eng, need, skip_self=(eng == "tensor"))
        self.ecnt[eng] += 1
        c = self.ecnt[eng]
        self.q[eng].append(("op", fn, c))
        self._record((("e", eng), c), reads, writes)
        self.n_ops += 1

    def dma(self, eng, out, in_, reads=(), writes=(), **kw):
        reads, writes = self._rw(reads, writes)
        need = self._deps(reads, writes)
        i = self.pnext
        self.pnext = (self.pnext + 1) % self.NPOOL
        sid = ("p", i)
        if self.pcnt[i] > 0:
            need[sid] = max(need.get(sid, 0), 16 * self.pcnt[i])
        self._emit_waits(eng, need)
        self.pcnt[i] += 1
        val = 16 * self.pcnt[i]
        self.q[eng].append(("dma", out, in_, kw, sid))
        self._record((sid, val), reads, writes)
        self.n_ops += 1
        return (sid, val)

    def final_wait(self, eng, events):
        need = {}
        for sid, val in events:
            need[sid] = max(need.get(sid, 0), val)
        self._emit_waits(eng, need)

    def emit(self, block):
        prog = self

        def runner(ename):
            items = prog.q[ename]

            def body(e):
                for it in items:
                    if it[0] == "wait":
                        e.wait_ge(prog.sems[it[1]], it[2])
                    elif it[0] == "op":
                        it[1](e).then_inc(prog.esem[ename], 1)
                    else:
                        _, out, in_, kw, sid = it
                        e.dma_start(out=out, in_=in_, **kw).then_inc(prog.sems[sid], 16)
            return body

        block.sync(runner("sync"))
        block.scalar(runner("scalar"))
        block.vector(runner("vector"))
        block.gpsimd(runner("gpsimd"))
        block.tensor(runner("tensor"))
        self.q = {e: [] for e in ENGS}


class K:
    def __init__(self, P):
        self.P = P

    def tt(self, eng, out, in0, in1, op, r, w):
        self.P.op(eng, lambda e: e.tensor_tensor(out=out, in0=in0, in1=in1, op=op), r, w)

    def ts(self, eng, out, in0, s1, op0, r, w, s2=None, op1=None):
        if op1 is None:
            self.P.op(eng, lambda e: e.tensor_scalar(out=out, in0=in0, scalar1=s1, scalar2=None, op0=op0), r, w)
        else:
            self.P.op(eng, lambda e: e.tensor_scalar(out=out, in0=in0, scalar1=s1, scalar2=s2, op0=op0, op1=op1), r, w)

    def stt(self, out, in0, sc, in1, op0, op1, r, w):
        self.P.op("vector", lambda e: e.scalar_tensor_tensor(out=out, in0=in0, scalar=sc, in1=in1, op0=op0, op1=op1), r, w)

    def act(self, out, in_, func, r, w, bias=None, scale=None):
        kw = {}
        if bias is not None:
            kw["bias"] = bias
        if scale is not None:
            kw["scale"] = scale
        self.P.op("scalar", lambda e: e.activation(out=out, in_=in_, func=func, **kw), r, w)

    def cp(self, eng, out, in_, r, w):
        if eng == "scalar":
            self.P.op("scalar", lambda e: e.copy(out=out, in_=in_), r, w)
        else:
            self.P.op(eng, lambda e: e.tensor_copy(out=out, in_=in_), r, w)

    def mm(self, out, lhsT, rhs, start, stop, r, w):
        self.P.op("tensor", lambda e: e.matmul(out, lhsT=lhsT, rhs=rhs, start=start, stop=stop), r, w)

    def tr(self, out, in_, ident, r, w):
        self.P.op("tensor", lambda e: e.transpose(out=out, in_=in_, identity=ident), r, w)

    def memset(self, eng, ap, val, w):
        self.P.op(eng, lambda e: e.memset(ap, val), (), w)


def build_nc(dbg=False, phases=("setup", "mixer", "peer")):
    nc = bass.Bass("TRN2", target_bir_lowering=False)
    D = {}
    di = lambda n, s, dt=F32: nc.dram_tensor(n, list(s), dt, kind="ExternalInput").ap()
    xs = di("xs", [4096, 1024])
    w_perm = di("w_perm", [1024, NWCOLS])
    mu_perm = di("mu_perm", [128, NMU])
    pv_d = di("pv", [128, NPV])
    lora_d = di("lora_w", [128, 512])
    gup_d = di("g_up", [128, 512])
    prw_d = di("p_rw", [512, 1024])
    pml_d = di("p_ml", [512, 1024])
    wout_d = di("w_out", [1024, 1024])
    mlnw_d = di("ml_norm_w", [1, 512])
    wq_d = di("peer_w_q", [1024, 2048])
    skT_d = di("skT", [128, 16 * 128])
    uT_d = di("uT", [128, 128, 1024])
    vp_d = di("vp", [128, 128, 1024])
    fg_d = di("final_g", [1, 1024])
    out_d = nc.dram_tensor("out", [2048, 1024], F32, kind="ExternalOutput").ap()
    wsc = nc.dram_tensor("wsc", [NPIECE, 128, 4096], BF16, kind="Internal").ap()
    x1_d = nc.dram_tensor("x1s", [2048, 1024], F32, kind="Internal").ap()
    G_d = nc.dram_tensor("Gs", [128, 128, 2048], BF16, kind="Internal").ap()
    if dbg:
        dbg_yml = nc.dram_tensor("dbg_yml", [2048, 512], F32, kind="ExternalOutput").ap()
        dbg_yrw = nc.dram_tensor("dbg_yrw", [512, 2048], F32, kind="ExternalOutput").ap()
        dbg_x1 = nc.dram_tensor("dbg_x1", [2048, 1024], F32, kind="ExternalOutput").ap()

    out_events = []
    with ExitStack() as gst:
        P = Prog(nc, gst)
        k = K(P)
        GT = lambda n, s, d: gst.enter_context(nc.sbuf_tensor("sb_" + n, s, d))
        pv = GT("pv", [128, NPV], F32)
        ident = GT("ident", [128, 128], BF16)
        identf = GT("identf", [128, 128], F32)
        pvc = lambda n, j=0: pv[:, PV[n] + j:PV[n] + j + 1]

        with ExitStack() as st:
            T = lambda n, s, d: st.enter_context(nc.sbuf_tensor("sb_" + n, s, d))
            P.dma("sync", pv[:], pv_d, writes=["pv"])
            k.memset("gpsimd", identf[:], 0.0, ["identf"])
            P.op("gpsimd", lambda e: e.affine_select(out=identf[:], in_=identf[:], pattern=[[-1, 128]], compare_op=ALU.not_equal, fill=1.0, base=0, channel_multiplier=1), ["identf"], ["identf"])
            k.cp("vector", ident[:], identf[:], ["identf"], ["ident"])
            if "setup" in phases:
                mub = T("mub", [128, NMU], F32)
                omub = T("omub", [128, NMU], F32)
                wst = [T("wst%d" % i, [128, 8, 512], F32) for i in range(2)]
                wtmp = T("wtmp", [128, 8, 512], F32)
                wob = [T("wob%d" % i, [128, 8, 512], BF16) for i in range(2)]
                P.dma("sync", mub[:], mu_perm, writes=["mub"])
                k.ts("vector", omub[:], mub[:], -1.0, ALU.mult, ["mub"], ["omub"], s2=1.0, op1=ALU.add)
                wv = w_perm.rearrange("(j p) n -> p j n", p=128)
                g1b = lambda n: pv[:, PV["g1"]:PV["g1"] + 8].unsqueeze(2).to_broadcast([128, 8, n])
                for pi, (pn, cols) in enumerate(PIECES):
                    _, off, n = PIDX[pn]
                    s = pi % 2
                    P.dma("sync", wst[s][:, :, 0:n], wv[:, :, off:off + n], writes=["wst%d" % s])
                    if pn.startswith("rw") or pn == "lo":
                        k.tt("vector", wtmp[:, :, 0:n], wst[s][:, :, 0:n], g1b(n), ALU.mult, ["wst%d" % s, "pv"], ["wtmp"])
                        if pn.startswith("rwA"):
                            segs = [(0, n, omub)]
                        elif pn.startswith("rwB"):
                            segs = [(0, n, mub)]
                        else:
                            segs = [(0, 256, omub), (256, 512, mub)]
                        for (a, b_, mt) in segs:
                            mv = mt[:, off + a:off + b_].unsqueeze(1).to_broadcast([128, 8, b_ - a])
                            k.tt("gpsimd", wob[s][:, :, a:b_], wtmp[:, :, a:b_], mv, ALU.mult, ["wtmp", "mub", "omub"], ["wob%d" % s])
                    else:
                        k.tt("vector", wob[s][:, :, 0:n], wst[s][:, :, 0:n], g1b(n), ALU.mult, ["wst%d" % s, "pv"], ["wob%d" % s])
                    dst = wsc[pi][:, 0:8 * n].rearrange("p (j n) -> p j n", j=8)
                    P.dma("sync", dst, wob[s][:, :, 0:n], reads=["wob%d" % s], writes=["wsc%d" % pi])
                for pi, src in [(PI_PRW, prw_d.rearrange("(j p) n -> p j n", p=128)),
                                (PI_PML, pml_d.rearrange("(j p) n -> p j n", p=128)),
                                (PI_WO0, wout_d.rearrange("(j p) n -> p j n", p=128)[:, :, 0:512]),
                                (PI_WO1, wout_d.rearrange("(j p) n -> p j n", p=128)[:, :, 512:1024])]:
                    s = pi % 2
                    if pi in (PI_PRW, PI_PML):
                        sv = wst[s][:].rearrange("p j n -> p (j n)").rearrange("p (j n) -> p j n", j=4)
                        ov = wob[s][:].rearrange("p j n -> p (j n)").rearrange("p (j n) -> p j n", j=4)
                        dv = wsc[pi].rearrange("p (d j n) -> p j d n", d=8, j=4)
                        ov = ov.rearrange("p j (d n) -> p j d n", d=8)
                        sv2 = sv
                    else:
                        sv, ov = wst[s][:], wob[s][:]
                        dv = wsc[pi].rearrange("p (j n) -> p j n", j=8)
                    P.dma("sync", sv, src, writes=["wst%d" % s])
                    k.cp("vector", wob[s][:], wst[s][:], ["wst%d" % s], ["wob%d" % s])
                    if pi in (PI_PRW, PI_PML):
                        for j in range(4):
                            P.dma("sync", dv[:, j], ov[:, j], reads=["wob%d" % s], writes=["wsc%d" % pi])
                    else:
                        P.dma("sync", dv, ov, reads=["wob%d" % s], writes=["wsc%d" % pi])
            with nc.Block() as block:
                P.emit(block)

        if "mixer" in phases:
            with ExitStack() as st:
                _mixer(nc, P, k, st, locals())
                with nc.Block() as block:
                    P.emit(block)

        if "peer" in phases:
            with ExitStack() as st:
                _peer(nc, P, k, st, locals())
        else:
            with ExitStack() as st:
                tl = st.enter_context(nc.sbuf_tensor("tl", [128, 1024], F32))
                for ti in range(16):
                    P.dma("sync", tl[:], x1_d[ti * 128:(ti + 1) * 128, :], reads=["x1d%d" % ti], writes=["tl"])
                    out_events.append(P.dma("sync", out_d[ti * 128:(ti + 1) * 128, :], tl[:], reads=["tl"]))
                P.final_wait("sync", out_events)
                with nc.Block() as block:
                    P.emit(block)
    return nc


def _mixer(nc, P, k, st, G):
    pv, ident, identf, pvc = G["pv"], G["ident"], G["identf"], G["pvc"]
    xs, wsc, x1_d, dbg = G["xs"], G["wsc"], G["x1_d"], G["dbg"]
    T = lambda n, s, d: st.enter_context(nc.sbuf_tensor("sb_" + n, s, d))
    PS = lambda n, s, d: st.enter_context(nc.psum_tensor("pp_" + n, s, d))
    for _b, _subs in {"ps0": ["ps0L", "ps0R"], "ps1": ["ps1L"], "ps2": ["ps2a", "ps2b", "ps2c"], "ps3": ["ps3a", "ps3b", "ps3c"],
                      "ps4": ["ps4_0", "ps4_1"], "ps5": ["ps5_0", "ps5_1"], "ps6": ["ps6_0", "ps6_1"], "pb": ["pbh"]}.items():
        for _s in _subs:
            P.alias[_s] = _b
    P.excl.update(["ps%d" % i for i in range(8)] + ["pb"])
    P.children.update({"CT": ["CT%d" % i for i in range(4)], "CTb": ["CTb%d" % i for i in range(4)],
                       "ST": ["ST%d" % i for i in range(4)], "STb": ["STb%d" % i for i in range(4)]})
    bdm = T("bdm", [128, 128], BF16)
    bdf = T("bdf", [128, 128], F32)
    bo64 = T("bo64", [128, 128], F32)
    MUs = T("MUs", [128, 128], F32)
    MUst = T("MUst", [128, 64], F32)
    MLs = T("MLs", [128, 128], F32)
    tri = T("tri", [128, 128], F32)
    hm = T("hm", [128, 2], F32)
    rm64 = T("rm64", [128, 512], F32)
    rm128 = T("rm128", [128, 512], F32)
    sel8 = T("sel8", [8, 8, 128], F32)
    mlnw = T("mlnw", [128, 512], F32)
    negs = T("negs", [128, 20], F32)
    ones1 = negs[:, 12:13]
    k.memset("gpsimd", bdf[:], 0.0, ["bdf"])
    k.memset("gpsimd", bdf[0:64, 0:64], 1.0, ["bdf"])
    k.memset("gpsimd", bdf[64:128, 64:128], 1.0, ["bdf"])
    k.cp("vector", bdm[:], bdf[:], ["bdf"], ["bdm"])
    k.ts("vector", bo64[:], bdf[:], 1.0 / 64.0, ALU.mult, ["bdf"], ["bo64"])
    k.memset("gpsimd", hm[:], 0.0, ["hm"])
    k.memset("gpsimd", hm[0:64, 0:1], 1.0, ["hm"])
    k.memset("gpsimd", hm[64:128, 1:2], 1.0, ["hm"])
    P.op("gpsimd", lambda e: e.affine_select(out=MUs[:], in_=bdf[:], pattern=[[1, 128]], compare_op=ALU.is_gt, fill=0.0, base=0, channel_multiplier=-1), ["bdf"], ["MUs"])
    P.op("gpsimd", lambda e: e.affine_select(out=MLs[:], in_=bdf[:], pattern=[[-1, 128]], compare_op=ALU.is_gt, fill=0.0, base=0, channel_multiplier=1), ["bdf"], ["MLs"])
    k.memset("gpsimd", tri[:], 1.0, ["tri"])
    P.op("gpsimd", lambda e: e.affine_select(out=tri[:], in_=tri[:], pattern=[[1, 128]], compare_op=ALU.is_ge, fill=0.0, base=0, channel_multiplier=-1), ["tri"], ["tri"])
    k.memset("gpsimd", MUst[:], 1.0, ["MUst"])
    for hh in range(2):
        P.op("gpsimd", lambda e, hh=hh: e.affine_select(out=MUst[hh * 64:(hh + 1) * 64, :], in_=MUst[hh * 64:(hh + 1) * 64, :], pattern=[[1, 64]], compare_op=ALU.is_ge, fill=0.0, base=0, channel_multiplier=-1), ["MUst"], ["MUst"])
    k.memset("gpsimd", rm64[:], 1.0, ["rm64"])
    k.memset("gpsimd", rm64[:].rearrange("p (c l) -> p c l", l=64)[:, :, 0:1], 0.0, ["rm64"])
    k.memset("gpsimd", rm128[:], 1.0, ["rm128"])
    k.memset("gpsimd", rm128[:].rearrange("p (c l) -> p c l", l=128)[:, :, 0:1], 0.0, ["rm128"])
    k.cp("vector", sel8[:], identf[0:8, 0:8].unsqueeze(2).to_broadcast([8, 8, 128]), ["identf"], ["sel8"])
    P.dma("sync", mlnw[:], G["mlnw_d"].partition_broadcast(128), writes=["mlnw"])
    k.ts("vector", negs[:, 0:4], pv[:, PV["w0"]:PV["w0"] + 4], -1.0, ALU.mult, ["pv"], ["negs"])
    k.ts("vector", negs[:, 4:8], pv[:, PV["k_a"]:PV["k_a"] + 4], -1.0, ALU.mult, ["pv"], ["negs"], s2=1.0, op1=ALU.add)
    k.ts("vector", negs[:, 8:12], pv[:, PV["b_f"]:PV["b_f"] + 4], -1.0, ALU.mult, ["pv"], ["negs"])
    k.memset("vector", negs[:, 12:13], 1.0, ["negs"])
    k.memset("vector", negs[:, 13:14], -0.5, ["negs"])
    k.memset("vector", negs[:, 14:15], 0.0, ["negs"])
    k.memset("vector", negs[:, 15:16], 1e-6, ["negs"])
    k.memset("vector", negs[:, 16:17], 1e-5, ["negs"])
    k.memset("vector", negs[:, 17:18], 64e-5, ["negs"])
    loraw = T("loraw", [128, 512], BF16)
    gupw = T("gupw", [128, 512], BF16)
    CT = T("CT", [128, 4, 129], F32)
    CTb = T("CTb", [128, 4, 129], BF16)
    ST = T("ST", [128, 4, 128], F32)
    STb = T("STb", [128, 4, 128], BF16)
    qkcar = T("qkcar", [128, 8, 3], F32)
    for (t_, nm) in [(CT, "CT"), (CTb, "CTb"), (ST, "ST"), (STb, "STb"), (qkcar, "qkcar")]:
        k.memset("vector", t_[:], 0.0, [nm])
    xT = T("xT", [128, 8, 513], BF16)
    k.memset("vector", xT[:, :, 0:1], 0.0, ["xT"])
    wb = [T("wb%d" % i, [128, 4096], BF16) for i in range(3)]
    wbn = [0]

    def load_piece(pi, nelem, extra=()):
        s = wbn[0] % 3
        wbn[0] += 1
        P.dma("gpsimd", wb[s][:, 0:nelem], wsc[pi][:, 0:nelem], reads=["wsc%d" % pi], writes=["wb%d" % s])
        for (pj, so, ne, do) in extra:
            P.dma("gpsimd", wb[s][:, do:do + ne], wsc[pj][:, so:so + ne], reads=["wsc%d" % pj], writes=["wb%d" % s])
        return wb[s], "wb%d" % s

    NT = 14
    Tt = [T("T%d" % i, [128, 512], F32) for i in range(NT)]
    tn = lambda i: "T%d" % i
    for (dst, src, nm) in [(loraw, G["lora_d"], "loraw"), (gupw, G["gup_d"], "gupw")]:
        P.dma("sync", Tt[0][:], src, writes=[tn(0)])
        k.cp("vector", dst[:], Tt[0][:], [tn(0)], [nm])
    xt = [T("xt0", [128, 1024], F32)] * 2
    xnb = T("xnb", [128, 1024], BF16)
    sm = T("sm", [128, 16], F32)
    rows8 = Tt[7][0:8, :]
    pre = [T("pre%d" % i, [128, 515], F32) for i in range(2)]
    qkt = T("qkt", [128, 8, 512], BF16)
    qt = qkt[:, 0:4, :]
    kt = qkt[:, 4:8, :]
    EGs = T("EGs", [128, 4, 4], F32)
    vaug = T("vaug", [128, 4, 4, 129], BF16)
    sigo = T("sigo", [128, 4, 512], BF16)
    ymlt = sigo
    ymlT = T("ymlT", [128, 4, 512], BF16)
    yrwT = T("yrwT", [128, 4, 512], BF16)
    ktok = [T("ktok%d" % i, [128, 128], BF16) for i in range(2)]
    PTt = [T("PT%d" % i, [128, 128], BF16) for i in range(2)]
    hh_ = [T("hh%d" % i, [128, 128], F32) for i in range(2)]
    st6 = T("st6", [128, 8], F32)
    tmpC = T("tmpC", [128, 129], F32)
    k.memset("vector", vaug[:, :, :, 128:129], 1.0, ["vaug"])
    twa = T("twa", [128, 512], BF16)
    sgz = T("sgz", [128, 512], BF16)
    ARbd = [T("ARbd%d" % i, [128, 8, 192], BF16) for i in range(2)]
    BKbd = [T("BKbd%d" % i, [128, 8, 256], BF16) for i in range(2)]
    VbT = [T("VbT%d" % i, [128, 8, 128], BF16) for i in range(2)]
    WLs = [T("WLs%d" % i, [128, 8], F32) for i in range(2)]
    XM = [[T("XM%d_%d" % (a, i), [128, 192], BF16) for i in range(8)] for a in range(2)]
    XTb = [T("XTb%d" % i, [128, 128], BF16) for i in range(8)]
    PA = [T("PA%d" % i, [128, 256], BF16) for i in range(8)]
    PB = [T("PB%d" % i, [128, 256], BF16) for i in range(8)]
    TTb = [[T("TTb%d_%d" % (a, i), [128, 128], BF16) for i in range(8)] for a in range(2)]
    M2 = [[T("M2_%d_%d" % (a, i), [128, 192], BF16) for i in range(8)] for a in range(2)]
    TOK = [[T("TOK%d_%d" % (a, i), [128, 3, 128], BF16) for i in range(8)] for a in range(2)]
    TgT = [T("TgT%d" % i, [128, 512], F32) for i in range(2)]
    vTb2 = [T("vTb%d" % i, [128, 512], BF16) for i in range(2)]
    pbon2 = [T("pbon%d" % i, [128, 512], BF16) for i in range(2)]
    RHSb = [T("RHSb%d" % i, [128, 128], BF16) for i in range(2)]
    Usb = [T("Usb%d" % i, [128, 128], BF16) for i in range(2)]
    MU192 = T("MU192", [128, 192], F32)
    tmpS = T("tmpS", [128, 128], F32)
    mergedT = qkt
    MKEYS = ["qt%d" % i for i in range(4)] + ["kt%d" % i for i in range(4)]
    k.cp("vector", MU192[:, 0:128], MUs[:], ["MUs"], ["MU192"])
    k.cp("vector", MU192[:, 128:192], MUst[:], ["MUst"], ["MU192"])
    pb = PS("pb", [128, 1024], BF16)
    ps = [PS("ps%d" % i, [128, 512], F32) for i in range(7)]
    pn = lambda i: "ps%d" % i

    def dump(name, ap, key):
        if not DUMP:
            return
        dt_ = nc.dram_tensor("dmp_" + name, list(ap.shape), ap.dtype, kind="ExternalOutput").ap()
        P.dma("sync", dt_, ap, reads=[key])

    wq = lambda buf, j, a, b_, n: buf[:, 0:8 * n].rearrange("p (j n) -> p j n", j=8)[:, j, a:b_]

    for blk in BLKS:
        own = blk >= 4
        for tt in range(4):
            lt = blk * 4 + tt
            xs_ = xt[lt % 2]
            xk = "xt0"
            P.dma("sync", xs_[:], xs[lt * 128:(lt + 1) * 128, :], writes=[xk])
            k.memset("vector", sm[:, 0:1], 0.0, ["sm0"])
            P.op("scalar", lambda e, xs_=xs_: e.activation(out=Tt[0][:].bitcast(BF16), in_=xs_[:], func=AF.Square, scale=1.0 / 32.0, accum_out=sm[:, 0:1]), [xk, "sm0"], [tn(0), "sm0"])
            k.act(sm[:, 1:2], sm[:, 0:1], AF.Ln, ["sm0", "negs"], ["sm1"], bias=negs[:, 15:16])
            k.act(sm[:, 1:2], sm[:, 1:2], AF.Exp, ["sm1"], ["sm1"], scale=-0.5)
            k.ts("vector", xnb[:], xs_[:], sm[:, 1:2], ALU.mult, [xk, "sm1"], ["xnb"])
            for j in range(8):
                k.tr(pb[:, j * 128:(j + 1) * 128], xnb[:, j * 128:(j + 1) * 128], ident[:], ["xnb", "ident"], ["pb"])
            k.cp("scalar", xT[:, :, 1 + tt * 128:1 + (tt + 1) * 128], pb[:].rearrange("p (j n) -> p j n", j=8), ["pb"], ["xT"])
        if blk == BLKS[-1]:
            dump("xT", xT[:], "xT")
            dump("sm", sm[:], "sm1")
            dump("xnb", xnb[:], "xnb")
        XC = lambda j: xT[:, j, 1:513]
        XP = lambda j: xT[:, j, 0:512]

        if 'B' in STAGES:
            wbuf, wk = load_piece(PIDX["mlif"][0], 8 * 8)
            for j in range(8):
                k.mm(ps[0][0:8, :], wq(wbuf, j, 0, 8, 8), XC(j), j == 0, j == 7, [wk, "xT"], [pn(0)])
            k.cp("vector", rows8[:], ps[0][0:8, :], [pn(0)], ["T7"])
            for h in range(4):
                k.mm(ps[1][:], sel8[:, h, :], rows8[:], True, True, ["sel8", "T7"], [pn(1)])
                k.mm(ps[2][:], sel8[:, 4 + h, :], rows8[:], True, True, ["sel8", "T7"], [pn(2)])
                k.act(Tt[1][:], ps[2][:], AF.Exp, [pn(2), "negs"], [tn(1)], bias=negs[:, 8 + h:9 + h], scale=-1.0)
                k.act(Tt[1][:], Tt[1][:], AF.Ln, [tn(1), "negs"], [tn(1)], bias=ones1)
                P.op("vector", lambda e: e.tensor_tensor_scan(out=Tt[2][:], data0=rm128[:], data1=Tt[1][:], initial=0.0, op0=ALU.mult, op1=ALU.add), [tn(1), "rm128"], [tn(2)])
                k.act(Tt[6][:], Tt[2][:], AF.Exp, [tn(2)], [tn(6)], scale=-1.0)
                k.cp("vector", EGs[:, h, :], Tt[6][:].rearrange("p (c l) -> p c l", l=128)[:, :, 127], [tn(6)], ["EGs%d" % h])
                k.tt("vector", Tt[3][:], ps[1][:], Tt[2][:], ALU.add, [pn(1), tn(2)], [tn(3)])
                k.act(Tt[3][:], Tt[3][:], AF.Exp, [tn(3), "pv"], [tn(3)], bias=pvc("b_i", h))
                wbuf, wk = load_piece(PIDX["mlqk%d" % h][0], 8 * 256)
                for qk in range(2):
                    pp = ps[3 + qk]
                    for j in range(8):
                        k.mm(pp[:], wq(wbuf, j, qk * 128, (qk + 1) * 128, 256), XC(j), j == 0, j == 7, [wk, "xT"], [pn(3 + qk)])
                    pr = pre[qk]
                    prk = "pre%d" % qk
                    ci = qk * 4 + h
                    k.cp("vector", pr[:, 0:3], qkcar[:, ci, :], ["qkcar"], [prk])
                    k.cp("scalar", pr[:, 3:515], pp[:], [pn(3 + qk)], [prk])
                    k.cp("vector", qkcar[:, ci, :], pr[:, 512:515], [prk], ["qkcar"])
                    wn = "cq_w" if qk == 0 else "ck_w"
                    bn = "cq_b" if qk == 0 else "ck_b"
                    acc = Tt[4 + qk]
                    ak = tn(4 + qk)
                    k.ts("vector", acc[:], pr[:, 0:512], pvc(wn, 0 * 4 + h), ALU.mult, [prk, "pv"], [ak])
                    for tap in range(1, 4):
                        k.stt(acc[:], pr[:, tap:tap + 512], pvc(wn, tap * 4 + h), acc[:], ALU.mult, ALU.add, [prk, "pv", ak], [ak])
                    k.act(acc[:], acc[:], AF.Silu, [ak, "pv"], [ak], bias=pvc(bn, h))
                    if qk == 0:
                        k.tt("vector", qt[:, h, :], acc[:], Tt[6][:], ALU.mult, [ak, tn(6)], ["qt%d" % h])
                    else:
                        k.stt(kt[:, h, :], acc[:], 128.0 ** -0.5, Tt[3][:], ALU.mult, ALU.mult, [ak, tn(3)], ["kt%d" % h])
            if blk == BLKS[-1]:
                dump("rows8", rows8[:], "T7")
                dump("EGs", EGs[:], "EGs3")
                dump("qt", qkt[:], "qt3")
                pass
                dump("T3", Tt[3][:], tn(3))
                dump("T4", Tt[4][:], tn(4))
                dump("T5", Tt[5][:], tn(5))
            wbuf, wk = load_piece(PIDX["mlv"][0], 8 * 512)
            for tt in range(4):
                pp = ps[tt % 2]
                for j in range(8):
                    k.mm(pp[:], xT[:, j, 1 + tt * 128:1 + (tt + 1) * 128], wq(wbuf, j, 0, 512, 512), j == 0, j == 7, [wk, "xT"], [pn(tt % 2)])
                k.cp("scalar", vaug[:, tt, :, 0:128], pp[:].rearrange("p (h n) -> p h n", h=4), [pn(tt % 2)], ["vaug"])
            if own:
                wbuf, wk = load_piece(PIDX["mlo"][0], 8 * 512)
                for tt in range(4):
                    pp = ps[2 + tt % 2]
                    for j in range(8):
                        k.mm(pp[:], xT[:, j, 1 + tt * 128:1 + (tt + 1) * 128], wq(wbuf, j, 0, 512, 512), j == 0, j == 7, [wk, "xT"], [pn(2 + tt % 2)])
                    k.act(sigo[:, tt, :], pp[:], AF.Sigmoid, [pn(2 + tt % 2)], ["sigo"])
            for tt in range(4):
                for h in range(4):
                    u = (tt * 4 + h) % 2
                    csl = slice(tt * 128, (tt + 1) * 128)
                    k.tr(pb[:, u * 128:(u + 1) * 128], kt[:, h, csl], ident[:], ["kt%d" % h, "ident"], ["pb"])
                    k.cp("scalar", ktok[u][:], pb[:, u * 128:(u + 1) * 128], ["pb"], ["ktok%d" % u])
                    k.mm(ps[4][:, u * 128:(u + 1) * 128], kt[:, h, csl], qt[:, h, csl], True, True, ["kt%d" % h, "qt%d" % h], ["ps4_%d" % u])
                    k.tt("vector", PTt[u][:], ps[4][:, u * 128:(u + 1) * 128], tri[:], ALU.mult, ["ps4_%d" % u, "tri"], ["PT%d" % u])
                    if own:
                        pN = ps[5][:, u * 256:u * 256 + 129]
                        pk = "ps5_%d" % u
                        k.mm(pN, qt[:, h, csl], CTb[:, h, :], True, False, ["qt%d" % h, "CTb%d" % h], [pk])
                        k.mm(pN, PTt[u][:], vaug[:, tt, h, :], False, True, ["PT%d" % u, "vaug"], [pk])
                        k.act(sm[:, 4:5], ps[5][:, u * 256 + 128:u * 256 + 129], AF.Abs, [pk], ["sm4"])
                        k.ts("vector", sm[:, 4:5], sm[:, 4:5], 1.0, ALU.max, ["sm4"], ["sm4"])
                        P.op("vector", lambda e: e.reciprocal(out=sm[:, 5:6], in_=sm[:, 4:5]), ["sm4"], ["sm5"])
                        k.ts("vector", hh_[u][:], ps[5][:, u * 256:u * 256 + 128], sm[:, 5:6], ALU.mult, [pk, "sm5"], ["hh%d" % u])
                        P.op("vector", lambda e, u=u: e.bn_stats(out=st6[:, 0:6], in_=hh_[u][:]), ["hh%d" % u], ["st6"])
                        P.op("vector", lambda e: e.bn_aggr(out=sm[:, 6:8], in_=st6[:, 0:6]), ["st6"], ["sm6"])
                        k.act(sm[:, 8:9], sm[:, 7:8], AF.Ln, ["sm6", "negs"], ["sm8"], bias=negs[:, 16:17])
                        k.act(sm[:, 8:9], sm[:, 8:9], AF.Exp, ["sm8"], ["sm8"], scale=-0.5)
                        k.ts("vector", hh_[u][:], hh_[u][:], sm[:, 6:7], ALU.subtract, ["hh%d" % u, "sm6", "sm8"], ["hh%d" % u], s2=sm[:, 8:9], op1=ALU.mult)
                        k.tt("vector", hh_[u][:], hh_[u][:], mlnw[:, h * 128:(h + 1) * 128], ALU.mult, ["hh%d" % u, "mlnw"], ["hh%d" % u])
                        k.tt("vector", ymlt[:, tt, h * 128:(h + 1) * 128], hh_[u][:], sigo[:, tt, h * 128:(h + 1) * 128], ALU.mult, ["hh%d" % u, "sigo"], ["sigo"])
                    pU = ps[6][:, u * 256:u * 256 + 129]
                    k.mm(pU, ktok[u][:], vaug[:, tt, h, :], True, True, ["ktok%d" % u, "vaug"], ["ps6_%d" % u])
                    sc = 1.0 if own else pvc("flag")
                    k.stt(tmpC[:], pU, sc, CT[:, h, :], ALU.mult, ALU.add, ["ps6_%d" % u, "CT%d" % h, "pv"], ["tmpC"])
                    k.ts("vector", CT[:, h, :], tmpC[:], EGs[:, h, tt:tt + 1], ALU.mult, ["tmpC", "EGs%d" % h], ["CT%d" % h])
                    k.cp("scalar", CTb[:, h, :], CT[:, h, :], ["CT%d" % h], ["CTb%d" % h])
            if blk == BLKS[-1]:
                dump("vaug", vaug[:], "vaug")
                dump("CT", CT[:], "CT")
                dump("ymlt", ymlt[:], "sigo")
                dump("sigo", sigo[:], "sigo")
            if own:
                for tt in range(4):
                    for wc in range(4):
                        k.tr(pb[:, 512 + wc * 128:512 + (wc + 1) * 128], ymlt[:, tt, wc * 128:(wc + 1) * 128], ident[:], ["sigo", "ident"], ["pbh"])
                    k.cp("scalar", ymlT[:, :, tt * 128:(tt + 1) * 128], pb[:, 512:1024].rearrange("p (j n) -> p j n", j=4), ["pbh"], ["ymlT"])
                if dbg:
                    ob = (blk - 4) * 512
                    for tt in range(4):
                        k.cp("vector", Tt[0][:], ymlt[:, tt, :], ["sigo"], [tn(0)])
                        P.dma("sync", G["dbg_yml"][ob + tt * 128:ob + (tt + 1) * 128, :], Tt[0][:], reads=[tn(0)])

        if 'C' in STAGES:
            wbuf, wk = load_piece(PIDX["lo"][0], 8 * 512)
            for (pi_, c0) in [(0, 0), (1, 128)]:
                for j in range(8):
                    k.mm(ps[pi_][:], wq(wbuf, j, c0, c0 + 128, 512), XC(j), j == 0, False, [wk, "xT"], [pn(pi_)])
                for j in range(8):
                    k.mm(ps[pi_][:], wq(wbuf, j, 256 + c0, 256 + c0 + 128, 512), XP(j), False, j == 7, [wk, "xT"], [pn(pi_)])
            k.act(twa[0:64, :], ps[0][0:64, :], AF.Tanh, [pn(0)], ["twa"])
            k.cp("vector", twa[64:128, :], ps[0][64:128, :], [pn(0)], ["twa"])
            k.act(sgz[:], ps[1][:], AF.Sigmoid, [pn(1)], ["sgz"])
            hmb = hm[:].unsqueeze(1).unsqueeze(3).to_broadcast([128, 8, 2, 64])
            bc4 = lambda t_: t_[:].rearrange("p (c s) -> p c s", s=64).unsqueeze(2).to_broadcast([128, 8, 2, 64])
            Tr, Tk, Tew, Tcs, Twi, Twv, Twe, Ta, T3, T4, T5, Tkp = [Tt[i] for i in range(12)]
            nr, nk, new, ncs, nwi, nwv, nwe, na, n3, n4, n5, nkp = [tn(i) for i in range(12)]

            def hpbuf(hp):
                s2 = hp % 2
                return (s2, ARbd[s2], BKbd[s2], VbT[s2], WLs[s2], "ARbd%d" % s2, "BKbd%d" % s2, "VbT%d" % s2, "WLs%d" % s2)

            def prep(hp):
                s2, AR, BK, VB, WL, nAR, nBK, nVB, nWL = hpbuf(hp)
                Tg, ng = TgT[s2], "TgT%d" % s2
                vT_, nvT = vTb2[s2], "vTb%d" % s2
                pb_, npb = pbon2[s2], "pbon%d" % s2
                wA, wAk = load_piece(PIDX["rwA%d" % hp][0], 8 * 384)
                wB, wBk = load_piece(PIDX["rwB%d" % hp][0], 8 * 384)
                for ci in range(3):
                    for j in range(8):
                        k.mm(ps[ci][:], wq(wA, j, ci * 128, (ci + 1) * 128, 384), XC(j), j == 0, False, [wAk, "xT"], [pn(ci)])
                    for j in range(8):
                        k.mm(ps[ci][:], wq(wB, j, ci * 128, (ci + 1) * 128, 384), XP(j), False, j == 7, [wBk, "xT"], [pn(ci)])
                hs = slice(hp * 128, (hp + 1) * 128)
                k.mm(ps[5][:], loraw[0:64, hs], twa[0:64, :], True, True, ["loraw", "twa"], [pn(5)])
                k.mm(ps[6][:], loraw[64:128, hs], twa[64:128, :], True, True, ["loraw", "twa"], [pn(6)])
                k.cp("scalar", Tr[:], ps[0][:], [pn(0)], [nr])
                k.cp("scalar", Tk[:], ps[1][:], [pn(1)], [nk])
                k.cp("vector", vT_[:], ps[2][:], [pn(2)], [nvT])
                k.act(T5[:], ps[5][:], AF.Exp, [pn(5), "negs"], [n5], bias=negs[:, hp:hp + 1], scale=-1.0)
                k.act(T5[:], T5[:], AF.Ln, [n5, "negs"], [n5], bias=ones1)
                k.act(Tew[:], T5[:], AF.Exp, [n5, "negs"], [new], bias=negs[:, 13:14], scale=-1.0)
                P.op("vector", lambda e: e.tensor_tensor_scan(out=Tt[3][:], data0=rm64[:], data1=Tt[2][:], initial=0.0, op0=ALU.mult, op1=ALU.add), [new, "rm64"], [ncs])
                k.act(Twi[:], Tcs[:], AF.Exp, [ncs], [nwi], scale=-1.0)
                k.act(Twv[:], Tcs[:], AF.Exp, [ncs], [nwv])
                k.tt("vector", T5[:], Tcs[:], Tew[:], ALU.subtract, [ncs, new], [n5])
                k.act(Twe[:], T5[:], AF.Exp, [n5], [nwe], scale=-1.0)
                k.act(Ta[:], ps[6][:], AF.Sigmoid, [pn(6), "pv"], [na], bias=pvc("a0", hp))
                if own:
                    k.mm(ps[0][:], gupw[:, hs], sgz[:], True, True, ["gupw", "sgz"], [pn(0)])
                    k.cp("scalar", Tg[:], ps[0][:], [pn(0)], [ng])
                k.ts("vector", T3[:], Tk[:], pvc("k_k", hp), ALU.mult, [nk, "pv"], [n3])
                k.tt("vector", T4[:], T3[:], T3[:], ALU.mult, [n3], [n4])
                k.mm(ps[1][:], bdf[:], T4[:], True, True, ["bdf", n4], [pn(1)])
                k.ts("vector", T4[:], ps[1][:], 1e-18, ALU.max, [pn(1)], [n4])
                k.act(T4[:], T4[:], AF.Ln, [n4], [n4])
                k.act(T4[:], T4[:], AF.Exp, [n4], [n4], scale=-0.5)
                k.tt("vector", T3[:], T3[:], T4[:], ALU.mult, [n3, n4], [n3])
                k.ts("vector", T4[:], Ta[:], pvc("k_a", hp), ALU.mult, [na, "pv", "negs"], [n4], s2=negs[:, 4 + hp:5 + hp], op1=ALU.add)
                k.tt("vector", Tkp[:], Tk[:], T4[:], ALU.mult, [nk, n4], [nkp])
                k.tt("vector", T4[:], T3[:], Ta[:], ALU.mult, [n3, na], [n4])
                k.stt(T5[:], T3[:], -1.0, Twe[:], ALU.mult, ALU.mult, [n3, nwe], [n5])
                k.tt("vector", AR[:, :, 0:128].rearrange("p c (h s) -> p c h s", h=2), bc4(T5), hmb, ALU.mult, [n5, "hm"], [nAR])
                k.tt("vector", AR[:, :, 128:192], Tr[:].rearrange("p (c s) -> p c s", s=64), Twi[:].rearrange("p (c s) -> p c s", s=64), ALU.mult, [nr, nwi], [nAR])
                k.tt("vector", T4[:], T4[:], Twv[:], ALU.mult, [n4, nwv], [n4])
                k.tt("gpsimd", BK[:, :, 0:128].rearrange("p c (h s) -> p c h s", h=2), bc4(T4), hmb, ALU.mult, [n4, "hm"], [nBK])
                k.tt("vector", T5[:], Tkp[:], Twv[:], ALU.mult, [nkp, nwv], [n5])
                k.tt("gpsimd", BK[:, :, 128:256].rearrange("p c (h s) -> p c h s", h=2), bc4(T5), hmb, ALU.mult, [n5, "hm"], [nBK])
                k.tt("gpsimd", VB[:].rearrange("p c (h s) -> p c h s", h=2), vT_[:].rearrange("p (c s) -> p c s", s=64).unsqueeze(2).to_broadcast([128, 8, 2, 64]), hmb, ALU.mult, [nvT, "hm"], [nVB])
                k.cp("vector", WL[:], Twi[:].rearrange("p (c s) -> p c s", s=64)[:, :, 63], [nwi], [nWL])
                if own:
                    k.stt(pb_[:], Tr[:], pvc("r_k", hp), Tkp[:], ALU.mult, ALU.mult, [nr, nkp, "pv"], [npb])

            SQB = [0, 1, 2]

            def steps_gen(hp):
                s2, AR, BK, VB, WL, nAR, nBK, nVB, nWL = hpbuf(hp)
                XM_, TT_, M2_, TOK_ = XM[s2], TTb[s2], M2[s2], TOK[s2]
                kx = lambda nm, c: "%s%d_%d" % (nm, s2, c)
                for c in range(NCH):
                    bA, bB = (0, 1) if c % 2 == 0 else (2, 5)
                    A_bd = AR[:, c, 0:128]
                    B_bd = BK[:, c, 0:128]
                    K_bd = BK[:, c, 128:256]
                    k.mm(ps[bA][:, 0:192], B_bd, AR[:, c, :], True, True, [nBK, nAR], [pn(bA)])
                    k.mm(ps[bA][:, 256:384], A_bd, B_bd, True, True, [nBK, nAR], [pn(bA)])
                    k.mm(ps[bB][:, 0:192], K_bd, AR[:, c, :], True, True, [nBK, nAR], [pn(bB)])
                    yield
                    k.tt("vector", XM_[c][:], ps[bA][:, 0:192], MU192[:], ALU.mult, [pn(bA), "MU192"], [kx("XM", c)])
                    k.tt("vector", XTb[c][:], ps[bA][:, 256:384], MLs[:], ALU.mult, [pn(bA), "MLs"], ["XTb%d" % c])
                    yield
                    k.tt("vector", M2_[c][:], ps[bB][:, 0:192], MU192[:], ALU.mult, [pn(bB), "MU192"], [kx("M2", c)])
                    k.tt("gpsimd", TT_[c][:], XM_[c][:, 0:128], ident[:], ALU.add, [kx("XM", c), "ident"], [kx("TT", c)])
                    yield
                for lvl in range(1, 6):
                    for c in range(NCH):
                        if lvl == 1:
                            cur, curT, kcur = XM_[c][:, 0:128], XTb[c][:], [kx("XM", c), "XTb%d" % c]
                        else:
                            src = PA[c] if lvl % 2 == 0 else PB[c]
                            cur, curT, kcur = src[:, 128:256], src[:, 0:128], [("PA%d" if lvl % 2 == 0 else "PB%d") % c]
                        dst = PA[c] if lvl % 2 == 1 else PB[c]
                        kdst = ("PA%d" if lvl % 2 == 1 else "PB%d") % c
                        bk = SQB[c % 3]
                        k.mm(ps[bk][:, 0:128], cur, curT, True, True, kcur, [pn(bk)])
                        if lvl < 5:
                            k.mm(ps[bk][:, 128:256], curT, cur, True, True, kcur, [pn(bk)])
                        wdt = 256 if lvl < 5 else 128
                        k.cp("scalar" if c % 2 else "vector", dst[:, 0:wdt], ps[bk][:, 0:wdt], [pn(bk)], [kdst])
                        yield
                    for c in range(NCH):
                        dst = PA[c] if lvl % 2 == 1 else PB[c]
                        kdst = ("PA%d" if lvl % 2 == 1 else "PB%d") % c
                        ba = 5 + c % 2
                        k.mm(ps[ba][:, 0:128], dst[:, 0:128], TT_[c][:], True, True, [kdst, kx("TT", c)], [pn(ba)])
                        k.tt("vector", TT_[c][:], ps[ba][:, 0:128], TT_[c][:], ALU.add, [pn(ba), kx("TT", c)], [kx("TT", c)])
                        yield
                for c in range(NCH):
                    k.tr(pb[:, 0:128], BK[:, c, 0:128], ident[:], [nBK, "ident"], ["pb"])
                    k.tr(pb[:, 128:256], BK[:, c, 128:256], ident[:], [nBK, "ident"], ["pb"])
                    k.tr(pb[:, 256:384], VB[:, c, :], ident[:], [nVB, "ident"], ["pb"])
                    k.cp("scalar", TOK_[c][:], pb[:, 0:384].rearrange("p (a n) -> p a n", a=3), ["pb"], [kx("TOK", c)])
                    yield

            def chain_gen(hp):
                s2, AR, BK, VB, WL, nAR, nBK, nVB, nWL = hpbuf(hp)
                XM_, TT_, M2_, TOK_ = XM[s2], TTb[s2], M2[s2], TOK[s2]
                kx = lambda nm, c: "%s%d_%d" % (nm, s2, c)
                for c in range(NCH):
                    d = c % 2
                    A_bd = AR[:, c, 0:128]
                    R_st = AR[:, c, 128:192]
                    Btok, Ktok_, Vbd = TOK_[c][:, 0, :], TOK_[c][:, 1, :], TOK_[c][:, 2, :]
                    k.mm(ps[3][:, 0:128], A_bd, STb[:, hp, :], True, False, [nAR, "STb%d" % hp], ["ps3a"])
                    k.mm(ps[3][:, 0:128], M2_[c][:, 0:128], Vbd, False, True, [kx("M2", c), kx("TOK", c)], ["ps3a"])
                    yield
                    k.cp("scalar", RHSb[d][:], ps[3][:, 0:128], ["ps3a"], ["RHSb%d" % d])
                    yield
                    k.mm(ps[3][:, 128:256], TT_[c][:], RHSb[d][:], True, True, [kx("TT", c), "RHSb%d" % d], ["ps3b"])
                    yield
                    k.cp("vector", Usb[d][:], ps[3][:, 128:256], ["ps3b"], ["Usb%d" % d])
                    yield
                    k.mm(ps[3][:, 256:384], Btok, Usb[d][:], True, False, [kx("TOK", c), "Usb%d" % d], ["ps3c"])
                    k.mm(ps[3][:, 256:384], Ktok_, Vbd, False, True, [kx("TOK", c)], ["ps3c"])
                    if own:
                        pY = ps[4][:, c * 64:(c + 1) * 64]
                        k.mm(pY, STb[:, hp, :], R_st, True, False, ["STb%d" % hp, nAR], [pn(4)])
                        k.mm(pY, Usb[d][:], XM_[c][:, 128:192], False, False, ["Usb%d" % d, kx("XM", c)], [pn(4)])
                        k.mm(pY, Vbd, M2_[c][:, 128:192], False, True, [kx("TOK", c), kx("M2", c)], [pn(4)])
                    yield
                    k.tt("vector", tmpS[:], ps[3][:, 256:384], ST[:, hp, :], ALU.add, ["ps3c", "ST%d" % hp], ["tmpS"])
                    yield
                    k.ts("gpsimd", STb[:, hp, :], tmpS[:], WL[:, c:c + 1], ALU.mult, ["tmpS", nWL], ["STb%d" % hp])
                    k.ts("vector", ST[:, hp, :], tmpS[:], WL[:, c:c + 1], ALU.mult, ["tmpS", nWL], ["ST%d" % hp])
                    yield

            def gn(hp):
                s2 = hp % 2
                Tg, ng = TgT[s2], "TgT%d" % s2
                vT_, nvT = vTb2[s2], "vTb%d" % s2
                pb_, npb = pbon2[s2], "pbon%d" % s2
                Y, Y2 = Tt[12], Tt[13]
                nY, nY2 = tn(12), tn(13)
                k.cp("scalar", Y[:], ps[4][:], [pn(4)], [nY])
                k.mm(ps[5][:], bo64[:], Y[:], True, True, ["bo64", nY], [pn(5)])
                k.tt("vector", Y[:], Y[:], ps[5][:], ALU.subtract, [nY, pn(5)], [nY])
                k.tt("vector", Y2[:], Y[:], Y[:], ALU.mult, [nY], [nY2])
                k.mm(ps[6][:], bo64[:], Y2[:], True, True, ["bo64", nY2], [pn(6)])
                k.act(Y2[:], ps[6][:], AF.Ln, [pn(6), "negs"], [nY2], bias=negs[:, 17:18])
                k.act(Y2[:], Y2[:], AF.Exp, [nY2], [nY2], scale=-0.5)
                k.tt("vector", Y[:], Y[:], Y2[:], ALU.mult, [nY, nY2], [nY])
                k.ts("vector", Y[:], Y[:], pvc("gn_w", hp), ALU.mult, [nY, "pv"], [nY], s2=pvc("gn_b", hp), op1=ALU.add)
                k.mm(ps[5][:], bdm[:], pb_[:], True, True, ["bdm", npb], [pn(5)])
                k.tt("vector", Y2[:], ps[5][:], vT_[:], ALU.mult, [pn(5), nvT], [nY2])
                k.tt("vector", Y[:], Y[:], Y2[:], ALU.add, [nY, nY2], [nY])
                k.tt("vector", yrwT[:, hp, :], Y[:], Tg[:], ALU.mult, [nY, ng], ["yrwT"])
                if dbg:
                    k.tt("vector", Y2[:], Y[:], Tg[:], ALU.mult, [nY, ng], [nY2])
                    ob = (blk - 4) * 512
                    P.dma("sync", G["dbg_yrw"][hp * 128:(hp + 1) * 128, ob:ob + 512], Y2[:], reads=[nY2])

            def drain(g):
                for _ in g:
                    pass

            def merge(ga, gb, ratio=3):
                doneb = False
                for _ in ga:
                    if not doneb:
                        for _r in range(ratio):
                            try:
                                next(gb)
                            except StopIteration:
                                doneb = True
                                break
                if not doneb:
                    drain(gb)

            prep(0)
            drain(steps_gen(0))
            for hp in range(NHP):
                if hp + 1 < NHP:
                    prep(hp + 1)
                    merge(chain_gen(hp), steps_gen(hp + 1))
                else:
                    drain(chain_gen(hp))
                if own:
                    gn(hp)

        if own and 'D' in STAGES:
            for dc in range(8):
                wg, wgk = load_piece(PIDX["gate%d" % dc][0], 8 * 256, extra=[(PI_PRW, dc * 512, 512, 2048), (PI_PML, dc * 512, 512, 2560)])
                o = 0 if dc % 2 == 0 else 3
                wP3 = wg[:, 2048:2560].rearrange("p (j n) -> p j n", j=4)
                wM3 = wg[:, 2560:3072].rearrange("p (j n) -> p j n", j=4)
                for wc in range(4):
                    k.mm(ps[o][:], wP3[:, wc, :], yrwT[:, wc, :], wc == 0, wc == 3, [wgk, "yrwT"], [pn(o)])
                for wc in range(4):
                    k.mm(ps[o + 1][:], wM3[:, wc, :], ymlT[:, wc, :], wc == 0, wc == 3, [wgk, "ymlT"], [pn(o + 1)])
                Ga, Gb = Tt[0], Tt[1]
                for gi in range(2):
                    for j in range(8):
                        k.mm(ps[o + 2][:], wq(wg, j, gi * 128, (gi + 1) * 128, 256), XC(j), j == 0, j == 7, [wgk, "xT"], [pn(o + 2)])
                    k.act([Ga, Gb][gi][:], ps[o + 2][:], AF.Sigmoid, [pn(o + 2), "pv"], [tn(gi)], bias=pvc("gate_b", gi * 8 + dc))
                k.tt("vector", Ga[:], Ga[:], ps[o][:], ALU.mult, [tn(0), pn(o)], [tn(0)])
                k.tt("vector", Gb[:], Gb[:], ps[o + 1][:], ALU.mult, [tn(1), pn(o + 1)], [tn(1)])
                k.tt("vector", mergedT[:, dc, :], Ga[:], Gb[:], ALU.add, [tn(0), tn(1)], MKEYS)
            w0_, w0k = load_piece(PI_WO0, 4096)
            w1_, w1k = load_piece(PI_WO1, 4096)
            for tt in range(4):
                lt = blk * 4 + tt
                ot = (blk - 4) * 4 + tt
                xs_ = xt[lt % 2]
                xk = "xt0"
                P.dma("sync", xs_[:], xs[lt * 128:(lt + 1) * 128, :], writes=[xk])
                for half, (wo, wok) in enumerate([(w0_, w0k), (w1_, w1k)]):
                    pp = ps[half + 2 * (tt % 2)]
                    ppk = pn(half + 2 * (tt % 2))
                    for dc in range(8):
                        k.mm(pp[:], mergedT[:, dc, tt * 128:(tt + 1) * 128], wq(wo, dc, 0, 512, 512), dc == 0, dc == 7, MKEYS + [wok], [ppk])
                    k.tt("vector", xs_[:, half * 512:(half + 1) * 512], xs_[:, half * 512:(half + 1) * 512], pp[:], ALU.add, [xk, ppk], [xk])
                P.dma("sync", x1_d[ot * 128:(ot + 1) * 128, :], xs_[:], reads=[xk], writes=["x1d%d" % ot])
                if dbg:
                    P.dma("sync", G["dbg_x1"][ot * 128:(ot + 1) * 128, :], xs_[:], reads=[xk])
        k.cp("vector", xT[:, :, 0:1], xT[:, :, 512:513], ["xT"], ["xT"])


def _peer(nc, P, k, st0, G):
    pv, ident, identf, pvc = G["pv"], G["ident"], G["identf"], G["pvc"]
    x1_d, G_d, out_d = G["x1_d"], G["G_d"], G["out_d"]
    g2col = lambda j: pv[:, PV["g2"] + j:PV["g2"] + j + 1]
    T0 = lambda n, s, d: st0.enter_context(nc.sbuf_tensor("sb_" + n, s, d))
    hn2T = T0("hn2T", [128, 8, 2048], BF16)
    eps6 = T0("eps6", [128, 1], F32)
    k.memset("vector", eps6[:], 1e-6, ["eps6"])
    P.alias.clear()
    P.excl.clear()
    P.excl.update(["q%d" % i for i in range(8)])
    NTI = len(PTILES)

    stA = ExitStack()
    abgT = stA.enter_context(nc.sbuf_tensor("sb_abgT", [128, 3, 2048], F32))
    with ExitStack() as st:
        T = lambda n, s, d: st.enter_context(nc.sbuf_tensor("sb_" + n, s, d))
        PS = lambda n, s, d: st.enter_context(nc.psum_tensor("pp_" + n, s, d))
        Wq = T("Wq", [128, 8, 2048], BF16)
        wst = T("wstq", [128, 8, 512], F32)
        skT = T("skT", [128, 16, 128], BF16)
        qT = T("qT", [128, 16, 512], BF16)
        s_all = T("s_all", [128, 16, 128], F32)
        work = T("work", [128, 128], F32)
        tops = T("tops", [128, 16, 16], F32)
        idx = T("idx", [128, 16, 16], U32)
        idxf = T("idxf", [128, 16, 16], F32)
        cand = T("cand", [128, 8, 256], F32)
        workc = T("workc", [128, 256], F32)
        best = T("best", [128, 8, 16], F32)
        pos = T("pos", [128, 8, 16], U32)
        pq = T("pq", [128, 2, 128], U32)
        pqf = T("pqf", [128, 2, 128], F32)
        eg = T("eg", [128, 8, 16], F32)
        zz = T("zz", [128, 16], F32)
        eq = T("eq", [128, 128, 16], F32)
        abg = T("abg", [128, 3, 128], F32)
        iota16 = T("iota16", [128, 16], F32)
        io32 = T("io32", [128, 16], I32)
        xt = [T("pxt%d" % i, [128, 1024], F32) for i in range(2)]
        xnb = T("pxnb", [128, 1024], BF16)
        junk = T("pjunk", [128, 1024], BF16)
        sm = T("psm", [128, 8], F32)
        pb = PS("qb", [128, 1024], BF16)
        ps = [PS("qs%d" % i, [128, 512], F32) for i in range(7)]
        pn = lambda i: "q%d" % i
        P.op("gpsimd", lambda e: e.iota(io32[:], pattern=[[1, 16]], base=0, channel_multiplier=0), (), ["io32"])
        k.cp("vector", iota16[:], io32[:], ["io32"], ["iota16"])
        wqv = G["wq_d"].rearrange("(j p) n -> p j n", p=128)
        g2b = pv[:, PV["g2"]:PV["g2"] + 8].unsqueeze(2).to_broadcast([128, 8, 512])
        for pc in range(4):
            P.dma("sync", wst[:], wqv[:, :, pc * 512:(pc + 1) * 512], writes=["wstq"])
            k.tt("vector", Wq[:, :, pc * 512:(pc + 1) * 512], wst[:], g2b, ALU.mult, ["wstq", "pv"], ["Wq"])
        P.dma("sync", wst[:].rearrange("p j n -> p (j n)")[:, 0:2048], G["skT_d"], writes=["wstq"])
        k.cp("vector", skT[:].rearrange("p g n -> p (g n)"), wst[:].rearrange("p j n -> p (j n)")[:, 0:2048], ["wstq"], ["skT"])
        for sti in range((NTI + 3) // 4):
            tiles = PTILES[sti * 4:(sti + 1) * 4]
            for tt, ti in enumerate(tiles):
                xs_ = xt[ti % 2]
                xk = "pxt%d" % (ti % 2)
                P.dma("sync", xs_[:], x1_d[ti * 128:(ti + 1) * 128, :], reads=["x1d%d" % ti], writes=[xk])
                k.memset("vector", sm[:, 0:1], 0.0, ["psm0"])
                P.op("scalar", lambda e, xs_=xs_: e.activation(out=junk[:], in_=xs_[:], func=AF.Square, scale=1.0 / 32.0, accum_out=sm[:, 0:1]), [xk, "psm0"], ["pjunk", "psm0"])
                k.act(sm[:, 1:2], sm[:, 0:1], AF.Ln, ["psm0", "eps6"], ["psm1"], bias=eps6[:])
                k.act(sm[:, 1:2], sm[:, 1:2], AF.Exp, ["psm1"], ["psm1"], scale=-0.5)
                k.ts("vector", xnb[:], xs_[:], sm[:, 1:2], ALU.mult, [xk, "psm1"], ["pxnb"])
                for j in range(8):
                    k.tr(pb[:, j * 128:(j + 1) * 128], xnb[:, j * 128:(j + 1) * 128], ident[:], ["pxnb", "ident"], ["q7"])
                k.cp("scalar", hn2T[:, :, ti * 128:(ti + 1) * 128], pb[:].rearrange("p (j n) -> p j n", j=8), ["q7"], ["hn2T"])
            nt = len(tiles) * 128
            c0 = tiles[0] * 128
            for g in range(16):
                pp = ps[g % 2]
                for j in range(8):
                    k.mm(pp[:, 0:nt], Wq[:, j, g * 128:(g + 1) * 128], hn2T[:, j, c0:c0 + nt], j == 0, j == 7, ["Wq", "hn2T"], [pn(g % 2)])
                k.cp("scalar" if g % 2 else "vector", qT[:, g, 0:nt], pp[:, 0:nt], [pn(g % 2)], ["qT"])
            for tt, ti in enumerate(tiles):
                for gg in range(4):
                    pp = ps[2 + gg % 2]
                    for g4 in range(4):
                        g = gg * 4 + g4
                        k.mm(pp[:, g4 * 128:(g4 + 1) * 128], qT[:, g, tt * 128:(tt + 1) * 128], skT[:, g, :], True, True, ["qT", "skT"], [pn(2 + gg % 2)])
                    k.cp("scalar", s_all[:, gg * 4:(gg + 1) * 4, :], pp[:].rearrange("p (g n) -> p g n", g=4), [pn(2 + gg % 2)], ["s_all"])
                V = "vector"
                for g in range(16):
                    P.op(V, lambda e, g=g: e.max(out=tops[:, g, 0:8], in_=s_all[:, g, :]), ["s_all"], ["tops"])
                    P.op(V, lambda e, g=g: e.max_index(out=idx[:, g, 0:8], in_max=tops[:, g, 0:8], in_values=s_all[:, g, :]), ["s_all", "tops"], ["idx"])
                    P.op(V, lambda e, g=g: e.match_replace(out=work[:], in_to_replace=tops[:, g, 0:8], in_values=s_all[:, g, :], imm_value=-1e30), ["s_all", "tops"], ["work"])
                    P.op(V, lambda e, g=g: e.max(out=tops[:, g, 8:16], in_=work[:]), ["work"], ["tops"])
                    P.op(V, lambda e, g=g: e.max_index(out=idx[:, g, 8:16], in_max=tops[:, g, 8:16], in_values=work[:]), ["work", "tops"], ["idx"])
                t4 = tops[:].rearrange("p (h q) i -> p h q i", q=2)
                k.tt(V, cand[:].rearrange("p h (i j) -> p h i j", j=16), t4[:, :, 0, :].unsqueeze(3).to_broadcast([128, 8, 16, 16]),
                     t4[:, :, 1, :].unsqueeze(2).to_broadcast([128, 8, 16, 16]), ALU.add, ["tops"], ["cand"])
                for h in range(8):
                    P.op(V, lambda e, h=h: e.max(out=best[:, h, 0:8], in_=cand[:, h, :]), ["cand"], ["best"])
                    P.op(V, lambda e, h=h: e.max_index(out=pos[:, h, 0:8], in_max=best[:, h, 0:8], in_values=cand[:, h, :]), ["cand", "best"], ["pos"])
                    P.op(V, lambda e, h=h: e.match_replace(out=workc[:], in_to_replace=best[:, h, 0:8], in_values=cand[:, h, :], imm_value=-1e30), ["cand", "best"], ["workc"])
                    P.op(V, lambda e, h=h: e.max(out=best[:, h, 8:16], in_=workc[:]), ["workc"], ["best"])
                    P.op(V, lambda e, h=h: e.max_index(out=pos[:, h, 8:16], in_max=best[:, h, 8:16], in_values=workc[:]), ["workc", "best"], ["pos"])
                k.tt(V, eg[:], best[:], best[:, :, 0:1].to_broadcast([128, 8, 16]), ALU.subtract, ["best"], ["eg"])
                k.act(eg[:], eg[:], AF.Exp, ["eg"], ["eg"])
                P.op(V, lambda e: e.tensor_reduce(out=zz[:, 0:8], in_=eg[:], axis=AX.X, op=ALU.add), ["eg"], ["zz"])
                P.op(V, lambda e: e.reciprocal(out=zz[:, 8:16], in_=zz[:, 0:8]), ["zz"], ["zz"])
                k.tt(V, abg[:, 2, :].rearrange("p (h n) -> p h n", h=8), eg[:], zz[:, 8:16].unsqueeze(2).to_broadcast([128, 8, 16]), ALU.mult, ["eg", "zz"], ["abg"])
                posf = pos[:].rearrange("p h n -> p (h n)")
                P.op(V, lambda e: e.tensor_single_scalar(out=pq[:, 0, :], in_=posf, scalar=4, op=ALU.logical_shift_right), ["pos"], ["pq"])
                P.op(V, lambda e: e.tensor_single_scalar(out=pq[:, 1, :], in_=posf, scalar=15, op=ALU.bitwise_and), ["pos"], ["pq"])
                k.cp(V, pqf[:], pq[:], ["pq"], ["pqf"])
                k.cp(V, idxf[:], idx[:], ["idx"], ["idxf"])
                i4 = idxf[:].rearrange("p (h q) i -> p h q i", q=2)
                for w_ in range(2):
                    k.tt(V, eq[:].rearrange("p (h n) i -> p h n i", h=8), iota16[:].unsqueeze(1).unsqueeze(1).to_broadcast([128, 8, 16, 16]),
                         pqf[:, w_, :].rearrange("p (h n) -> p h n", h=8).unsqueeze(3).to_broadcast([128, 8, 16, 16]), ALU.is_equal, ["iota16", "pqf"], ["eq"])
                    k.tt(V, eq[:].rearrange("p (h n) i -> p h n i", h=8), eq[:].rearrange("p (h n) i -> p h n i", h=8),
                         i4[:, :, w_, :].unsqueeze(2).to_broadcast([128, 8, 16, 16]), ALU.mult, ["eq", "idxf"], ["eq"])
                    P.op(V, lambda e, w_=w_: e.tensor_reduce(out=abg[:, w_, :], in_=eq[:], axis=AX.X, op=ALU.add), ["eq"], ["abg"])
                for w_ in range(3):
                    P.op("tensor", lambda e, w_=w_: e.transpose(out=ps[4][:, w_ * 128:(w_ + 1) * 128], in_=abg[:, w_, :], identity=identf[:]), ["abg", "identf"], [pn(4)])
                k.cp("scalar", abgT[:, :, ti * 128:(ti + 1) * 128], ps[4][:, 0:384].rearrange("p (w n) -> p w n", w=3), [pn(4)], ["abgT"])
        if G["dbg"]:
            dd = nc.dram_tensor("dbg_abgT", [128, 3, 2048], F32, kind="ExternalOutput").ap()
            P.dma("sync", dd, abgT[:], reads=["abgT"])
        with nc.Block() as block:
            P.emit(block)

    with ExitStack() as st:
        T = lambda n, s, d: st.enter_context(nc.sbuf_tensor("sb_" + n, s, d))
        PS = lambda n, s, d: st.enter_context(nc.psum_tensor("pp_" + n, s, d))
        iotak = T("iotak", [128, 128], F32)
        iok32 = T("iok32", [128, 128], I32)
        OA = [T("OA%d" % i, [128, 64, 128], BF16) for i in range(2)]
        WB = [T("WB%d" % i, [128, 64, 128], BF16) for i in range(2)]
        Gs = [T("Gs%d" % i, [128, 128, 128], BF16) for i in range(2)]
        ps = [PS("rs%d" % i, [128, 512], F32) for i in range(8)]
        pn = lambda i: "q%d" % i
        P.op("gpsimd", lambda e: e.iota(iok32[:], pattern=[[1, 128]], base=0, channel_multiplier=0), (), ["iok32"])
        k.cp("vector", iotak[:], iok32[:], ["iok32"], ["iotak"])
        ikb = iotak[:].unsqueeze(1).to_broadcast([128, 64, 128])
        nev = 0
        P.children["Gd"] = ["Gd%d_%d" % (ti, cq) for ti in range(16) for cq in range(4)]
        halves = [(ti, half) for ti in PTILES for half in range(2)]

        def build(ix, part):
            ti, half = halves[ix]
            hb = ix % 2
            t0 = ti * 128 + half * 64
            bc = lambda w_: abgT[:, w_, t0:t0 + 64].unsqueeze(2).to_broadcast([128, 64, 128])
            if part == 0:
                k.tt("vector", OA[hb][:], ikb, bc(0), ALU.is_equal, ["iotak", "abgT"], ["OA%d" % hb])
            else:
                k.tt("vector", WB[hb][:], ikb, bc(1), ALU.is_equal, ["iotak", "abgT"], ["WB%d" % hb])
                k.tt("gpsimd", WB[hb][:], WB[hb][:], bc(2), ALU.mult, ["WB%d" % hb, "abgT"], ["WB%d" % hb])

        build(0, 0)
        build(0, 1)
        for ix, (ti, half) in enumerate(halves):
            hb = ix % 2
            gsb = Gs[ti % 2]
            gk = "Gs%d" % (ti % 2)
            for tq in range(16):
                if ix + 1 < len(halves) and tq in (2, 9):
                    build(ix + 1, 0 if tq == 2 else 1)
                b_ = nev % 8
                nev += 1
                for t4 in range(4):
                    t = tq * 4 + t4
                    k.mm(ps[b_][:, t4 * 128:(t4 + 1) * 128], OA[hb][:, t, :], WB[hb][:, t, :], True, True, ["OA%d" % hb, "WB%d" % hb], [pn(b_)])
                tl = half * 64 + tq * 4
                k.cp("vector" if tq % 3 == 2 else "scalar", gsb[:, :, tl:tl + 4].rearrange("p c t -> p t c"), ps[b_][:].rearrange("p (t c) -> p t c", t=4), [pn(b_)], [gk])
            if half == 1:
                for cq in range(4):
                    P.dma("sync", G_d[cq * 32:(cq + 1) * 32, :, ti * 128:(ti + 1) * 128].rearrange("c k t -> k c t"), gsb[:, cq * 32:(cq + 1) * 32, :], reads=[gk], writes=["Gd%d_%d" % (ti, cq)])
        with nc.Block() as block:
            P.emit(block)

    stA.close()
    with ExitStack() as st:
        T = lambda n, s, d: st.enter_context(nc.sbuf_tensor("sb_" + n, s, d))
        PS = lambda n, s, d: st.enter_context(nc.psum_tensor("pp_" + n, s, d))
        GS = 2
        acc = T("acc", [128, 16, 1024], F32)
        fgb = T("fgb", [128, 1024], F32)
        ust = [T("ust%d" % i, [128, GS, 1024], F32) for i in range(2)]
        vst = [T("vst%d" % i, [128, GS, 1024], F32) for i in range(2)]
        ub = [T("ub%d" % i, [128, GS, 1024], BF16) for i in range(2)]
        vb = [T("vb%d" % i, [128, GS, 1024], BF16) for i in range(2)]
        gb = [T("gb%d" % i, [128, GS, 2048], BF16) for i in range(2)]
        gl = [T("gl%d" % i, [128, 512], BF16) for i in range(2)]
        amq = [T("amq%d" % i, [128, GS, 512], BF16) for i in range(2)]
        sm = T("fsm", [128, 8], F32)
        junk = T("fjunk", [128, 1024], BF16)
        pS = [PS("bS%d" % i, [128, 512], F32) for i in range(2)]
        pA = [PS("bA%d" % i, [128, 512], F32) for i in range(6)]
        P.dma("sync", fgb[:], G["fg_d"].partition_broadcast(128), writes=["fgb"])
        for ti in PTILES:
            P.dma("sync", acc[:, ti, :], x1_d[ti * 128:(ti + 1) * 128, :], reads=["x1d%d" % ti], writes=["acc%d" % ti])
        g2b4 = pv[:, PV["g2"]:PV["g2"] + 8].unsqueeze(1).unsqueeze(3).to_broadcast([128, GS, 8, 128])
        quads = [PTILES[i:i + 4] for i in range(0, len(PTILES), 4)]
        steps = [(grp, q) for grp in range(NGRP) for q in range(len(quads))]
        cnt = {"set": 0, "S": 0}

        def loads(grp):
            s = grp % 2
            c0 = grp * GS
            for i in range(GS):
                P.dma("sync", ust[s][:, i, :], G["uT_d"][c0 + i], writes=["ust%d" % s])
                P.dma("sync", vst[s][:, i, :], G["vp_d"][c0 + i], writes=["vst%d" % s])
                P.dma("sync", gb[s][:, i, :], G_d[c0 + i], reads=["Gd"], writes=["gb%d" % s])
            k.tt("gpsimd", ub[s][:].rearrange("p g (j n) -> p g j n", j=8), ust[s][:].rearrange("p g (j n) -> p g j n", j=8), g2b4, ALU.mult, ["ust%d" % s, "pv"], ["ub%d" % s])
            k.cp("gpsimd", vb[s][:], vst[s][:], ["vst%d" % s], ["vb%d" % s])

        def stage1(n):
            grp, q = steps[n]
            s = grp % 2
            pr = quads[q]
            t0 = pr[0] * 128
            nt = len(pr) * 128
            a_ = amq[n % 2]
            for i in range(GS):
                sb_ = cnt["S"] % 2
                cnt["S"] += 1
                for j in range(8):
                    k.mm(pS[sb_][:, 0:nt], ub[s][:, i, j * 128:(j + 1) * 128], hn2T[:, j, t0:t0 + nt], j == 0, j == 7, ["ub%d" % s, "hn2T"], ["q%d" % sb_])
                k.act(gl[sb_][:, 0:nt], pS[sb_][:, 0:nt], AF.Gelu, ["q%d" % sb_], ["gl%d" % sb_])
                k.tt("vector", a_[:, i, 0:nt], gl[sb_][:, 0:nt], gb[s][:, i, t0:t0 + nt], ALU.mult, ["gl%d" % sb_, "gb%d" % s], ["amq%d" % (n % 2)])

        def stage2(n):
            grp, q = steps[n]
            s = grp % 2
            pr = quads[q]
            a_ = amq[n % 2]
            for tt, ti in enumerate(pr):
                st_ = cnt["set"] % 3
                cnt["set"] += 1
                for hf in range(2):
                    bk = st_ * 2 + hf
                    for i in range(GS):
                        k.mm(pA[bk][:], a_[:, i, tt * 128:(tt + 1) * 128], vb[s][:, i, hf * 512:(hf + 1) * 512], i == 0, i == GS - 1, ["amq%d" % (n % 2), "vb%d" % s], ["q%d" % (2 + bk)])
                for hf in range(2):
                    bk = st_ * 2 + hf
                    k.tt("vector", acc[:, ti, hf * 512:(hf + 1) * 512], acc[:, ti, hf * 512:(hf + 1) * 512], pA[bk][:], ALU.add, ["acc%d" % ti, "q%d" % (2 + bk)], ["acc%d" % ti])

        loads(0)
        if NGRP > 1:
            loads(1)
        stage1(0)
        for n in range(len(steps)):
            if n + 1 < len(steps):
                stage1(n + 1)
            stage2(n)
            grp, q = steps[n]
            if q == len(quads) - 1 and grp + 2 < NGRP:
                loads(grp + 2)
        evs = []
        for ti in PTILES:
            ak = "acc%d" % ti
            k.memset("vector", sm[:, 0:1], 0.0, ["fsm0"])
            P.op("scalar", lambda e, ti=ti: e.activation(out=junk[:], in_=acc[:, ti, :], func=AF.Square, scale=1.0 / 32.0, accum_out=sm[:, 0:1]), [ak, "fsm0"], ["fjunk", "fsm0"])
            k.act(sm[:, 1:2], sm[:, 0:1], AF.Ln, ["fsm0", "eps6"], ["fsm1"], bias=eps6[:])
            k.act(sm[:, 1:2], sm[:, 1:2], AF.Exp, ["fsm1"], ["fsm1"], scale=-0.5)
            k.stt(acc[:, ti, :], acc[:, ti, :], sm[:, 1:2], fgb[:], ALU.mult, ALU.mult, [ak, "fsm1", "fgb"], [ak])
            evs.append(P.dma("sync", out_d[ti * 128:(ti + 1) * 128, :], acc[:, ti, :], reads=[ak]))
        P.final_wait("sync", evs)
        with nc.Block() as block:
            P.emit(block)


def _host_inputs(inp):
    f = lambda a: np.ascontiguousarray(np.asarray(a, dtype=np.float32))
    x = f(inp["x"])
    w_in = f(inp["w_in"])[0]
    colidx = np.concatenate([np.asarray(c, dtype=np.int64) for _, c in PIECES])
    w_perm = np.ascontiguousarray(w_in[:, colidx])
    mu = f(inp["rw_mu"])[0]
    mu_cols = mu[colidx[:NMU] - RW0]
    mu_perm = np.ascontiguousarray(np.broadcast_to(mu_cols[None, :], (128, NMU)))
    pvh = np.zeros((128, NPV), np.float32)
    ch = lambda v, n: np.asarray(v, np.float32).reshape(n, 128).T
    pvh[:, PV["g1"]:PV["g1"] + 8] = ch(inp["norm1_g"][0], 8)
    for nm, key in [("w0", "rw_w0"), ("a0", "rw_a0"), ("k_k", "rw_k_k"), ("k_a", "rw_k_a"), ("gn_w", "rw_gn_w"), ("gn_b", "rw_gn_b"),
                    ("cq_b", "ml_conv_q_b"), ("ck_b", "ml_conv_k_b")]:
        pvh[:, PV[nm]:PV[nm] + 4] = ch(inp[key][0], 4)
    pvh[:, PV["r_k"]:PV["r_k"] + 4] = ch(np.asarray(inp["rw_r_k"][0]).reshape(512), 4)
    for nm, key in [("cq_w", "ml_conv_q_w"), ("ck_w", "ml_conv_k_w")]:
        w = np.asarray(inp[key][0], np.float32)
        for tap in range(4):
            pvh[:, PV[nm] + tap * 4:PV[nm] + tap * 4 + 4] = ch(w[tap], 4)
    pvh[:, PV["gate_b"]:PV["gate_b"] + 16] = ch(inp["gate_b"][0], 16)
    pvh[:, PV["g2"]:PV["g2"] + 8] = ch(inp["norm2_g"][0], 8)
    pvh[:, PV["b_i"]:PV["b_i"] + 4] = np.asarray(inp["ml_b_i"][0], np.float32)[None, :]
    pvh[:, PV["b_f"]:PV["b_f"] + 4] = np.asarray(inp["ml_b_f"][0], np.float32)[None, :]
    lora = np.concatenate([f(inp["rw_w_up"])[0], f(inp["rw_a_up"])[0]], axis=0)
    sk = f(inp["peer_sub_keys"])[0]
    skT = np.ascontiguousarray(sk.reshape(16, 128, 128).transpose(2, 0, 1).reshape(128, 16 * 128))
    U = f(inp["peer_u"])[0]
    uT = np.ascontiguousarray(U.reshape(128, 128, 8, 128).transpose(1, 3, 2, 0).reshape(128, 128, 1024))
    V = f(inp["peer_v"])[0]
    vp = np.ascontiguousarray(V.reshape(128, 128, 1024).transpose(1, 0, 2))
    common = {
        "w_perm": w_perm, "mu_perm": mu_perm, "lora_w": np.ascontiguousarray(lora), "g_up": f(inp["rw_g_up"])[0],
        "p_rw": f(inp["p_rw"])[0], "p_ml": f(inp["p_ml"])[0], "w_out": f(inp["w_out"])[0],
        "ml_norm_w": f(inp["ml_norm_w"])[0][None, :], "peer_w_q": f(inp["peer_w_q"])[0], "skT": skT, "uT": uT, "vp": vp,
        "final_g": f(inp["final_g"])[None, :],
    }
    maps = []
    for c in range(8):
        b, half = c // 2, c % 2
        xs = np.zeros((4096, 1024), np.float32)
        if half == 0:
            xs[2048:] = x[b, :2048]
        else:
            xs[:] = x[b]
        pvc = pvh.copy()
        pvc[:, PV["flag"]] = float(half)
        m = dict(common)
        m["xs"] = xs
        m["pv"] = pvc
        maps.append(m)
    return maps


def kernel(**inputs):
    maps = _host_inputs(inputs)
    nc = build_nc()
    res = run_bass_kernel_spmd(nc, maps, core_ids=list(range(8)))
    out = np.zeros((4, 4096, 1024), np.float32)
    for c in range(8):
        b, half = c // 2, c % 2
        out[b, half * 2048:(half + 1) * 2048] = res.results[c]["out"]
    return out
```

```python
import numpy as np
from contextlib import ExitStack
import concourse.bass as bass
import concourse.mybir as mybir
from concourse.bass_utils import run_bass_kernel_spmd

F32 = mybir.dt.float32
BF16 = mybir.dt.bfloat16
U32 = mybir.dt.uint32
I32 = mybir.dt.int32
AF = mybir.ActivationFunctionType
ALU = mybir.AluOpType
AX = mybir.AxisListType

ENGS = ["sync", "scalar", "vector", "gpsimd", "tensor"]

RW0, ML0, GT0 = 0, 1792, 3848


def _piece_cols():
    pcs = []
    ar = lambda a, n: list(range(a, a + n))
    for ab in "AB":
        for hp in range(4):
            pcs.append(("rw%s%d" % (ab, hp), ar(RW0 + hp * 128, 128) + ar(RW0 + 512 + hp * 128, 128) + ar(RW0 + 1024 + hp * 128, 128)))
    pcs.append(("lo", ar(RW0 + 1536, 256) + ar(RW0 + 1536, 256)))
    for h in range(4):
        pcs.append(("mlqk%d" % h, ar(ML0 + h * 128, 128) + ar(ML0 + 512 + h * 128, 128)))
    pcs.append(("mlv", ar(ML0 + 1024, 512)))
    pcs.append(("mlo", ar(ML0 + 1536, 512)))
    pcs.append(("mlif", ar(ML0 + 2048, 8)))
    for dc in range(8):
        pcs.append(("gate%d" % dc, ar(GT0 + dc * 128, 128) + ar(GT0 + 1024 + dc * 128, 128)))
    return pcs


PIECES = _piece_cols()
PIDX = {}
_off = 0
for _i, (_n, _c) in enumerate(PIECES):
    PIDX[_n] = (_i, _off, len(_c))
    _off += len(_c)
NWCOLS = _off
N_INPIECE = len(PIECES)
PI_PRW = N_INPIECE
PI_PML = N_INPIECE + 1
PI_WO0 = N_INPIECE + 2
PI_WO1 = N_INPIECE + 3
NPIECE = N_INPIECE + 4
NMU = 1536 * 2 + 512

PV = {}
_o = 0
for _n, _k in [("g1", 8), ("w0", 4), ("a0", 4), ("k_k", 4), ("k_a", 4), ("r_k", 4), ("gn_w", 4), ("gn_b", 4),
               ("cq_w", 16), ("cq_b", 4), ("ck_w", 16), ("ck_b", 4), ("gate_b", 16), ("g2", 8), ("b_i", 4),
               ("b_f", 4), ("flag", 1)]:
    PV[_n] = _o
    _o += _k
NPV = _o


STAGES = "ABCD"
CCUT = 99
NCH = 8
NHP = 4
CSUB = 99
DUMP = False
PTILES = list(range(16))
NGRP = 64
BLKS = list(range(8))


class Prog:
    NPOOL = 24

    def __init__(self, nc, stack):
        self.nc = nc
        self.esem = {e: stack.enter_context(nc.semaphore("s_" + e)) for e in ENGS}
        self.ecnt = {e: 0 for e in ENGS}
        self.pool = [stack.enter_context(nc.semaphore("d%d" % i)) for i in range(self.NPOOL)]
        self.pcnt = [0] * self.NPOOL
        self.pnext = 0
        self.sems = {}
        for e in ENGS:
            self.sems[("e", e)] = self.esem[e]
        for i in range(self.NPOOL):
            self.sems[("p", i)] = self.pool[i]
        self.seen = {e: {} for e in ENGS}
        self.last_w = {}
        self.readers = {}
        self.n_ops = 0
        self.n_waits = 0
        self.q = {e: [] for e in ENGS}
        self.children = {}
        self.alias = {}
        self.excl = set()

    def _exp(self, keys):
        out = []
        for k_ in keys:
            k_ = self.alias.get(k_, k_)
            out.append(k_)
            out.extend(self.children.get(k_, ()))
        return tuple(out)

    def _rw(self, reads, writes):
        reads = self._exp(reads)
        writes = list(self._exp(writes))
        r2 = []
        for k_ in reads:
            if k_ in self.excl:
                if k_ not in writes:
                    writes.append(k_)
            else:
                r2.append(k_)
        return tuple(r2), tuple(writes)

    def _deps(self, reads, writes):
        need = {}
        for k in reads:
            ev = self.last_w.get(k)
            if ev is not None:
                need[ev[0]] = max(need.get(ev[0], 0), ev[1])
        for k in writes:
            ev = self.last_w.get(k)
            if ev is not None:
                need[ev[0]] = max(need.get(ev[0], 0), ev[1])
            for ev in self.readers.get(k, ()):
                need[ev[0]] = max(need.get(ev[0], 0), ev[1])
        return need

    def _emit_waits(self, eng, need, skip_self=False):
        for sid, val in need.items():
            if skip_self and sid == ("e", eng):
                continue
            if self.seen[eng].get(sid, 0) >= val:
                continue
            self.seen[eng][sid] = val
            self.q[eng].append(("wait", sid, val))
            self.n_waits += 1

    def _record(self, ev, reads, writes):
        for k in writes:
            self.last_w[k] = ev
            self.readers[k] = []
        for k in reads:
            if k in writes:
                continue
            self.readers.setdefault(k, []).append(ev)

    def op(self, eng, fn, reads=(), writes=()):
        reads, writes = self._rw(reads, writes)
        need = self._deps(reads, writes)
        self._emit_waits(eng, need, skip_self=(eng == "tensor"))
        self.ecnt[eng] += 1
        c = self.ecnt[eng]
        self.q[eng].append(("op", fn, c))
        self._record((("e", eng), c), reads, writes)
        self.n_ops += 1

    def dma(self, eng, out, in_, reads=(), writes=(), **kw):
        reads, writes = self._rw(reads, writes)
        need = self._deps(reads, writes)
        i = self.pnext
        self.pnext = (self.pnext + 1) % self.NPOOL
        sid = ("p", i)
        if self.pcnt[i] > 0:
            need[sid] = max(need.get(sid, 0), 16 * self.pcnt[i])
        self._emit_waits(eng, need)
        self.pcnt[i] += 1
        val = 16 * self.pcnt[i]
        self.q[eng].append(("dma", out, in_, kw, sid))
        self._record((sid, val), reads, writes)
        self.n_ops += 1
        return (sid, val)

    def final_wait(self, eng, events):
        need = {}
        for sid, val in events:
            need[sid] = max(need.get(sid, 0), val)
        self._emit_waits(eng, need)

    def emit(self, block):
        prog = self

        def runner(ename):
            items = prog.q[ename]

            def body(e):
                for it in items:
                    if it[0] == "wait":
                        e.wait_ge(prog.sems[it[1]], it[2])
                    elif it[0] == "op":
                        it[1](e).then_inc(prog.esem[ename], 1)
                    else:
                        _, out, in_, kw, sid = it
                        e.dma_start(out=out, in_=in_, **kw).then_inc(prog.sems[sid], 16)
            return body

        block.sync(runner("sync"))
        block.scalar(runner("scalar"))
        block.vector(runner("vector"))
        block.gpsimd(runner("gpsimd"))
        block.tensor(runner("tensor"))
        self.q = {e: [] for e in ENGS}


class K:
    def __init__(self, P):
        self.P = P

    def tt(self, eng, out, in0, in1, op, r, w):
        self.P.op(eng, lambda e: e.tensor_tensor(out=out, in0=in0, in1=in1, op=op), r, w)

    def ts(self, eng, out, in0, s1, op0, r, w, s2=None, op1=None):
        if op1 is None:
            self.P.op(eng, lambda e: e.tensor_scalar(out=out, in0=in0, scalar1=s1, scalar2=None, op0=op0), r, w)
        else:
            self.P.op(eng, lambda e: e.tensor_scalar(out=out, in0=in0, scalar1=s1, scalar2=s2, op0=op0, op1=op1), r, w)

    def stt(self, out, in0, sc, in1, op0, op1, r, w):
        self.P.op("vector", lambda e: e.scalar_tensor_tensor(out=out, in0=in0, scalar=sc, in1=in1, op0=op0, op1=op1), r, w)

    def act(self, out, in_, func, r, w, bias=None, scale=None):
        kw = {}
        if bias is not None:
            kw["bias"] = bias
        if scale is not None:
            kw["scale"] = scale
        self.P.op("scalar", lambda e: e.activation(out=out, in_=in_, func=func, **kw), r, w)

    def cp(self, eng, out, in_, r, w):
        if eng == "scalar":
            self.P.op("scalar", lambda e: e.copy(out=out, in_=in_), r, w)
        else:
            self.P.op(eng, lambda e: e.tensor_copy(out=out, in_=in_), r, w)

    def mm(self, out, lhsT, rhs, start, stop, r, w):
        self.P.op("tensor", lambda e: e.matmul(out, lhsT=lhsT, rhs=rhs, start=start, stop=stop), r, w)

    def tr(self, out, in_, ident, r, w):
        self.P.op("tensor", lambda e: e.transpose(out=out, in_=in_, identity=ident), r, w)

    def memset(self, eng, ap, val, w):
        self.P.op(eng, lambda e: e.memset(ap, val), (), w)


def build_nc(dbg=False, phases=("setup", "mixer", "peer")):
    nc = bass.Bass("TRN2", target_bir_lowering=False)
    D = {}
    di = lambda n, s, dt=F32: nc.dram_tensor(n, list(s), dt, kind="ExternalInput").ap()
    xs = di("xs", [4096, 1024])
    w_perm = di("w_perm", [1024, NWCOLS])
    mu_perm = di("mu_perm", [128, NMU])
    pv_d = di("pv", [128, NPV])
    lora_d = di("lora_w", [128, 512])
    gup_d = di("g_up", [128, 512])
    prw_d = di("p_rw", [512, 1024])
    pml_d = di("p_ml", [512, 1024])
    wout_d = di("w_out", [1024, 1024])
    mlnw_d = di("ml_norm_w", [1, 512])
    wq_d = di("peer_w_q", [1024, 2048])
    skT_d = di("skT", [128, 16 * 128])
    uT_d = di("uT", [128, 128, 1024])
    vp_d = di("vp", [128, 128, 1024])
    fg_d = di("final_g", [1, 1024])
    out_d = nc.dram_tensor("out", [2048, 1024], F32, kind="ExternalOutput").ap()
    wsc = nc.dram_tensor("wsc", [NPIECE, 128, 4096], BF16, kind="Internal").ap()
    x1_d = nc.dram_tensor("x1s", [2048, 1024], F32, kind="Internal").ap()
    G_d = nc.dram_tensor("Gs", [128, 128, 2048], BF16, kind="Internal").ap()
    if dbg:
        dbg_yml = nc.dram_tensor("dbg_yml", [2048, 512], F32, kind="ExternalOutput").ap()
        dbg_yrw = nc.dram_tensor("dbg_yrw", [512, 2048], F32, kind="ExternalOutput").ap()
        dbg_x1 = nc.dram_tensor("dbg_x1", [2048, 1024], F32, kind="ExternalOutput").ap()

    out_events = []
    with ExitStack() as gst:
        P = Prog(nc, gst)
        k = K(P)
        GT = lambda n, s, d: gst.enter_context(nc.sbuf_tensor("sb_" + n, s, d))
        pv = GT("pv", [128, NPV], F32)
        ident = GT("ident", [128, 128], BF16)
        identf = GT("identf", [128, 128], F32)
        pvc = lambda n, j=0: pv[:, PV[n] + j:PV[n] + j + 1]

        with ExitStack() as st:
            T = lambda n, s, d: st.enter_context(nc.sbuf_tensor("sb_" + n, s, d))
            P.dma("sync", pv[:], pv_d, writes=["pv"])
            k.memset("gpsimd", identf[:], 0.0, ["identf"])
            P.op("gpsimd", lambda e: e.affine_select(out=identf[:], in_=identf[:], pattern=[[-1, 128]], compare_op=ALU.not_equal, fill=1.0, base=0, channel_multiplier=1), ["identf"], ["identf"])
            k.cp("vector", ident[:], identf[:], ["identf"], ["ident"])
            if "setup" in phases:
                mub = T("mub", [128, NMU], F32)
                omub = T("omub", [128, NMU], F32)
                wst = [T("wst%d" % i, [128, 8, 512], F32) for i in range(2)]
                wtmp = T("wtmp", [128, 8, 512], F32)
                wob = [T("wob%d" % i, [128, 8, 512], BF16) for i in range(2)]
                P.dma("sync", mub[:], mu_perm, writes=["mub"])
                k.ts("vector", omub[:], mub[:], -1.0, ALU.mult, ["mub"], ["omub"], s2=1.0, op1=ALU.add)
                wv = w_perm.rearrange("(j p) n -> p j n", p=128)
                g1b = lambda n: pv[:, PV["g1"]:PV["g1"] + 8].unsqueeze(2).to_broadcast([128, 8, n])
                for pi, (pn, cols) in enumerate(PIECES):
                    _, off, n = PIDX[pn]
                    s = pi % 2
                    P.dma("sync", wst[s][:, :, 0:n], wv[:, :, off:off + n], writes=["wst%d" % s])
                    if pn.startswith("rw") or pn == "lo":
                        k.tt("vector", wtmp[:, :, 0:n], wst[s][:, :, 0:n], g1b(n), ALU.mult, ["wst%d" % s, "pv"], ["wtmp"])
                        if pn.startswith("rwA"):
                            segs = [(0, n, omub)]
                        elif pn.startswith("rwB"):
                            segs = [(0, n, mub)]
                        else:
                            segs = [(0, 256, omub), (256, 512, mub)]
                        for (a, b_, mt) in segs:
                            mv = mt[:, off + a:off + b_].unsqueeze(1).to_broadcast([128, 8, b_ - a])
                            k.tt("gpsimd", wob[s][:, :, a:b_], wtmp[:, :, a:b_], mv, ALU.mult, ["wtmp", "mub", "omub"], ["wob%d" % s])
                    else:
                        k.tt("vector", wob[s][:, :, 0:n], wst[s][:, :, 0:n], g1b(n), ALU.mult, ["wst%d" % s, "pv"], ["wob%d" % s])
                    dst = wsc[pi][:, 0:8 * n].rearrange("p (j n) -> p j n", j=8)
                    P.dma("sync", dst, wob[s][:, :, 0:n], reads=["wob%d" % s], writes=["wsc%d" % pi])
                for pi, src in [(PI_PRW, prw_d.rearrange("(j p) n -> p j n", p=128)),
                                (PI_PML, pml_d.rearrange("(j p) n -> p j n", p=128)),
                                (PI_WO0, wout_d.rearrange("(j p) n -> p j n", p=128)[:, :, 0:512]),
                                (PI_WO1, wout_d.rearrange("(j p) n -> p j n", p=128)[:, :, 512:1024])]:
                    s = pi % 2
                    if pi in (PI_PRW, PI_PML):
                        sv = wst[s][:].rearrange("p j n -> p (j n)").rearrange("p (j n) -> p j n", j=4)
                        ov = wob[s][:].rearrange("p j n -> p (j n)").rearrange("p (j n) -> p j n", j=4)
                        dv = wsc[pi].rearrange("p (d j n) -> p j d n", d=8, j=4)
                        ov = ov.rearrange("p j (d n) -> p j d n", d=8)
                        sv2 = sv
                    else:
                        sv, ov = wst[s][:], wob[s][:]
                        dv = wsc[pi].rearrange("p (j n) -> p j n", j=8)
                    P.dma("sync", sv, src, writes=["wst%d" % s])
                    k.cp("vector", wob[s][:], wst[s][:], ["wst%d" % s], ["wob%d" % s])
                    if pi in (PI_PRW, PI_PML):
                        for j in range(4):
                            P.dma("sync", dv[:, j], ov[:, j], reads=["wob%d" % s], writes=["wsc%d" % pi])
                    else:
                        P.dma("sync", dv, ov, reads=["wob%d" % s], writes=["wsc%d" % pi])
            with nc.Block() as block:
                P.emit(block)

        if "mixer" in phases:
            with ExitStack() as st:
                _mixer(nc, P, k, st, locals())
                with nc.Block() as block:
                    P.emit(block)

        if "peer" in phases:
            with ExitStack() as st:
                _peer(nc, P, k, st, locals())
        else:
            with ExitStack() as st:
                tl = st.enter_context(nc.sbuf_tensor("tl", [128, 1024], F32))
                for ti in range(16):
                    P.dma("sync", tl[:], x1_d[ti * 128:(ti + 1) * 128, :], reads=["x1d%d" % ti], writes=["tl"])
                    out_events.append(P.dma("sync", out_d[ti * 128:(ti + 1) * 128, :], tl[:], reads=["tl"]))
                P.final_wait("sync", out_events)
                with nc.Block() as block:
                    P.emit(block)
    return nc


def _mixer(nc, P, k, st, G):
    pv, ident, identf, pvc = G["pv"], G["ident"], G["identf"], G["pvc"]
    xs, wsc, x1_d, dbg = G["xs"], G["wsc"], G["x1_d"], G["dbg"]
    T = lambda n, s, d: st.enter_context(nc.sbuf_tensor("sb_" + n, s, d))
    PS = lambda n, s, d: st.enter_context(nc.psum_tensor("pp_" + n, s, d))
    for _b, _subs in {"ps0": ["ps0L", "ps0R"], "ps1": ["ps1L"], "ps2": ["ps2a", "ps2b", "ps2c"], "ps3": ["ps3a", "ps3b", "ps3c"],
                      "ps4": ["ps4_0", "ps4_1"], "ps5": ["ps5_0", "ps5_1"], "ps6": ["ps6_0", "ps6_1"], "pb": ["pbh"]}.items():
        for _s in _subs:
            P.alias[_s] = _b
    P.excl.update(["ps%d" % i for i in range(8)] + ["pb"])
    P.children.update({"CT": ["CT%d" % i for i in range(4)], "CTb": ["CTb%d" % i for i in range(4)],
                       "ST": ["ST%d" % i for i in range(4)], "STb": ["STb%d" % i for i in range(4)]})
    bdm = T("bdm", [128, 128], BF16)
    bdf = T("bdf", [128, 128], F32)
    bo64 = T("bo64", [128, 128], F32)
    MUs = T("MUs", [128, 128], F32)
    MUst = T("MUst", [128, 64], F32)
    MLs = T("MLs", [128, 128], F32)
    tri = T("tri", [128, 128], F32)
    hm = T("hm", [128, 2], F32)
    rm64 = T("rm64", [128, 512], F32)
    rm128 = T("rm128", [128, 512], F32)
    sel8 = T("sel8", [8, 8, 128], F32)
    mlnw = T("mlnw", [128, 512], F32)
    negs = T("negs", [128, 20], F32)
    ones1 = negs[:, 12:13]
    k.memset("gpsimd", bdf[:], 0.0, ["bdf"])
    k.memset("gpsimd", bdf[0:64, 0:64], 1.0, ["bdf"])
    k.memset("gpsimd", bdf[64:128, 64:128], 1.0, ["bdf"])
    k.cp("vector", bdm[:], bdf[:], ["bdf"], ["bdm"])
    k.ts("vector", bo64[:], bdf[:], 1.0 / 64.0, ALU.mult, ["bdf"], ["bo64"])
    k.memset("gpsimd", hm[:], 0.0, ["hm"])
    k.memset("gpsimd", hm[0:64, 0:1], 1.0, ["hm"])
    k.memset("gpsimd", hm[64:128, 1:2], 1.0, ["hm"])
    P.op("gpsimd", lambda e: e.affine_select(out=MUs[:], in_=bdf[:], pattern=[[1, 128]], compare_op=ALU.is_gt, fill=0.0, base=0, channel_multiplier=-1), ["bdf"], ["MUs"])
    P.op("gpsimd", lambda e: e.affine_select(out=MLs[:], in_=bdf[:], pattern=[[-1, 128]], compare_op=ALU.is_gt, fill=0.0, base=0, channel_multiplier=1), ["bdf"], ["MLs"])
    k.memset("gpsimd", tri[:], 1.0, ["tri"])
    P.op("gpsimd", lambda e: e.affine_select(out=tri[:], in_=tri[:], pattern=[[1, 128]], compare_op=ALU.is_ge, fill=0.0, base=0, channel_multiplier=-1), ["tri"], ["tri"])
    k.memset("gpsimd", MUst[:], 1.0, ["MUst"])
    for hh in range(2):
        P.op("gpsimd", lambda e, hh=hh: e.affine_select(out=MUst[hh * 64:(hh + 1) * 64, :], in_=MUst[hh * 64:(hh + 1) * 64, :], pattern=[[1, 64]], compare_op=ALU.is_ge, fill=0.0, base=0, channel_multiplier=-1), ["MUst"], ["MUst"])
    k.memset("gpsimd", rm64[:], 1.0, ["rm64"])
    k.memset("gpsimd", rm64[:].rearrange("p (c l) -> p c l", l=64)[:, :, 0:1], 0.0, ["rm64"])
    k.memset("gpsimd", rm128[:], 1.0, ["rm128"])
    k.memset("gpsimd", rm128[:].rearrange("p (c l) -> p c l", l=128)[:, :, 0:1], 0.0, ["rm128"])
    k.cp("vector", sel8[:], identf[0:8, 0:8].unsqueeze(2).to_broadcast([8, 8, 128]), ["identf"], ["sel8"])
    P.dma("sync", mlnw[:], G["mlnw_d"].partition_broadcast(128), writes=["mlnw"])
    k.ts("vector", negs[:, 0:4], pv[:, PV["w0"]:PV["w0"] + 4], -1.0, ALU.mult, ["pv"], ["negs"])
    k.ts("vector", negs[:, 4:8], pv[:, PV["k_a"]:PV["k_a"] + 4], -1.0, ALU.mult, ["pv"], ["negs"], s2=1.0, op1=ALU.add)
    k.ts("vector", negs[:, 8:12], pv[:, PV["b_f"]:PV["b_f"] + 4], -1.0, ALU.mult, ["pv"], ["negs"])
    k.memset("vector", negs[:, 12:13], 1.0, ["negs"])
    k.memset("vector", negs[:, 13:14], -0.5, ["negs"])
    k.memset("vector", negs[:, 14:15], 0.0, ["negs"])
    k.memset("vector", negs[:, 15:16], 1e-6, ["negs"])
    k.memset("vector", negs[:, 16:17], 1e-5, ["negs"])
    k.memset("vector", negs[:, 17:18], 64e-5, ["negs"])
    loraw = T("loraw", [128, 512], BF16)
    gupw = T("gupw", [128, 512], BF16)
    CT = T("CT", [128, 4, 129], F32)
    CTb = T("CTb", [128, 4, 129], BF16)
    ST = T("ST", [128, 4, 128], F32)
    STb = T("STb", [128, 4, 128], BF16)
    qkcar = T("qkcar", [128, 8, 3], F32)
    for (t_, nm) in [(CT, "CT"), (CTb, "CTb"), (ST, "ST"), (STb, "STb"), (qkcar, "qkcar")]:
        k.memset("vector", t_[:], 0.0, [nm])
    xT = T("xT", [128, 8, 513], BF16)
    k.memset("vector", xT[:, :, 0:1], 0.0, ["xT"])
    wb = [T("wb%d" % i, [128, 4096], BF16) for i in range(3)]
    wbn = [0]

    def load_piece(pi, nelem, extra=()):
        s = wbn[0] % 3
        wbn[0] += 1
        P.dma("gpsimd", wb[s][:, 0:nelem], wsc[pi][:, 0:nelem], reads=["wsc%d" % pi], writes=["wb%d" % s])
        for (pj, so, ne, do) in extra:
            P.dma("gpsimd", wb[s][:, do:do + ne], wsc[pj][:, so:so + ne], reads=["wsc%d" % pj], writes=["wb%d" % s])
        return wb[s], "wb%d" % s

    NT = 14
    Tt = [T("T%d" % i, [128, 512], F32) for i in range(NT)]
    tn = lambda i: "T%d" % i
    for (dst, src, nm) in [(loraw, G["lora_d"], "loraw"), (gupw, G["gup_d"], "gupw")]:
        P.dma("sync", Tt[0][:], src, writes=[tn(0)])
        k.cp("vector", dst[:], Tt[0][:], [tn(0)], [nm])
    xt = [T("xt0", [128, 1024], F32)] * 2
    xnb = T("xnb", [128, 1024], BF16)
    sm = T("sm", [128, 16], F32)
    rows8 = Tt[7][0:8, :]
    pre = [T("pre%d" % i, [128, 515], F32) for i in range(2)]
    qkt = T("qkt", [128, 8, 512], BF16)
    qt = qkt[:, 0:4, :]
    kt = qkt[:, 4:8, :]
    EGs = T("EGs", [128, 4, 4], F32)
    vaug = T("vaug", [128, 4, 4, 129], BF16)
    sigo = T("sigo", [128, 4, 512], BF16)
    ymlt = sigo
    ymlT = T("ymlT", [128, 4, 512], BF16)
    yrwT = T("yrwT", [128, 4, 512], BF16)
    ktok = [T("ktok%d" % i, [128, 128], BF16) for i in range(2)]
    PTt = [T("PT%d" % i, [128, 128], BF16) for i in range(2)]
    hh_ = [T("hh%d" % i, [128, 128], F32) for i in range(2)]
    st6 = T("st6", [128, 8], F32)
    tmpC = T("tmpC", [128, 129], F32)
    k.memset("vector", vaug[:, :, :, 128:129], 1.0, ["vaug"])
    twa = T("twa", [128, 512], BF16)
    sgz = T("sgz", [128, 512], BF16)
    ARbd = [T("ARbd%d" % i, [128, 8, 192], BF16) for i in range(2)]
    BKbd = [T("BKbd%d" % i, [128, 8, 256], BF16) for i in range(2)]
    VbT = [T("VbT%d" % i, [128, 8, 128], BF16) for i in range(2)]
    WLs = [T("WLs%d" % i, [128, 8], F32) for i in range(2)]
    XM = [[T("XM%d_%d" % (a, i), [128, 192], BF16) for i in range(8)] for a in range(2)]
    XTb = [T("XTb%d" % i, [128, 128], BF16) for i in range(8)]
    PA = [T("PA%d" % i, [128, 256], BF16) for i in range(8)]
    PB = [T("PB%d" % i, [128, 256], BF16) for i in range(8)]
    TTb = [[T("TTb%d_%d" % (a, i), [128, 128], BF16) for i in range(8)] for a in range(2)]
    M2 = [[T("M2_%d_%d" % (a, i), [128, 192], BF16) for i in range(8)] for a in range(2)]
    TOK = [[T("TOK%d_%d" % (a, i), [128, 3, 128], BF16) for i in range(8)] for a in range(2)]
    TgT = [T("TgT%d" % i, [128, 512], F32) for i in range(2)]
    vTb2 = [T("vTb%d" % i, [128, 512], BF16) for i in range(2)]
    pbon2 = [T("pbon%d" % i, [128, 512], BF16) for i in range(2)]
    RHSb = [T("RHSb%d" % i, [128, 128], BF16) for i in range(2)]
    Usb = [T("Usb%d" % i, [128, 128], BF16) for i in range(2)]
    MU192 = T("MU192", [128, 192], F32)
    tmpS = T("tmpS", [128, 128], F32)
    mergedT = qkt
    MKEYS = ["qt%d" % i for i in range(4)] + ["kt%d" % i for i in range(4)]
    k.cp("vector", MU192[:, 0:128], MUs[:], ["MUs"], ["MU192"])
    k.cp("vector", MU192[:, 128:192], MUst[:], ["MUst"], ["MU192"])
    pb = PS("pb", [128, 1024], BF16)
    ps = [PS("ps%d" % i, [128, 512], F32) for i in range(7)]
    pn = lambda i: "ps%d" % i

    def dump(name, ap, key):
        if not DUMP:
            return
        dt_ = nc.dram_tensor("dmp_" + name, list(ap.shape), ap.dtype, kind="ExternalOutput").ap()
        P.dma("sync", dt_, ap, reads=[key])

    wq = lambda buf, j, a, b_, n: buf[:, 0:8 * n].rearrange("p (j n) -> p j n", j=8)[:, j, a:b_]

    for blk in BLKS:
        own = blk >= 4
        for tt in range(4):
            lt = blk * 4 + tt
            xs_ = xt[lt % 2]
            xk = "xt0"
            P.dma("sync", xs_[:], xs[lt * 128:(lt + 1) * 128, :], writes=[xk])
            k.memset("vector", sm[:, 0:1], 0.0, ["sm0"])
            P.op("scalar", lambda e, xs_=xs_: e.activation(out=Tt[0][:].bitcast(BF16), in_=xs_[:], func=AF.Square, scale=1.0 / 32.0, accum_out=sm[:, 0:1]), [xk, "sm0"], [tn(0), "sm0"])
            k.act(sm[:, 1:2], sm[:, 0:1], AF.Ln, ["sm0", "negs"], ["sm1"], bias=negs[:, 15:16])
            k.act(sm[:, 1:2], sm[:, 1:2], AF.Exp, ["sm1"], ["sm1"], scale=-0.5)
            k.ts("vector", xnb[:], xs_[:], sm[:, 1:2], ALU.mult, [xk, "sm1"], ["xnb"])
            for j in range(8):
                k.tr(pb[:, j * 128:(j + 1) * 128], xnb[:, j * 128:(j + 1) * 128], ident[:], ["xnb", "ident"], ["pb"])
            k.cp("scalar", xT[:, :, 1 + tt * 128:1 + (tt + 1) * 128], pb[:].rearrange("p (j n) -> p j n", j=8), ["pb"], ["xT"])
        if blk == BLKS[-1]:
            dump("xT", xT[:], "xT")
            dump("sm", sm[:], "sm1")
            dump("xnb", xnb[:], "xnb")
        XC = lambda j: xT[:, j, 1:513]
        XP = lambda j: xT[:, j, 0:512]

        if 'B' in STAGES:
            wbuf, wk = load_piece(PIDX["mlif"][0], 8 * 8)
            for j in range(8):
                k.mm(ps[0][0:8, :], wq(wbuf, j, 0, 8, 8), XC(j), j == 0, j == 7, [wk, "xT"], [pn(0)])
            k.cp("vector", rows8[:], ps[0][0:8, :], [pn(0)], ["T7"])
            for h in range(4):
                k.mm(ps[1][:], sel8[:, h, :], rows8[:], True, True, ["sel8", "T7"], [pn(1)])
                k.mm(ps[2][:], sel8[:, 4 + h, :], rows8[:], True, True, ["sel8", "T7"], [pn(2)])
                k.act(Tt[1][:], ps[2][:], AF.Exp, [pn(2), "negs"], [tn(1)], bias=negs[:, 8 + h:9 + h], scale=-1.0)
                k.act(Tt[1][:], Tt[1][:], AF.Ln, [tn(1), "negs"], [tn(1)], bias=ones1)
                P.op("vector", lambda e: e.tensor_tensor_scan(out=Tt[2][:], data0=rm128[:], data1=Tt[1][:], initial=0.0, op0=ALU.mult, op1=ALU.add), [tn(1), "rm128"], [tn(2)])
                k.act(Tt[6][:], Tt[2][:], AF.Exp, [tn(2)], [tn(6)], scale=-1.0)
                k.cp("vector", EGs[:, h, :], Tt[6][:].rearrange("p (c l) -> p c l", l=128)[:, :, 127], [tn(6)], ["EGs%d" % h])
                k.tt("vector", Tt[3][:], ps[1][:], Tt[2][:], ALU.add, [pn(1), tn(2)], [tn(3)])
                k.act(Tt[3][:], Tt[3][:], AF.Exp, [tn(3), "pv"], [tn(3)], bias=pvc("b_i", h))
                wbuf, wk = load_piece(PIDX["mlqk%d" % h][0], 8 * 256)
                for qk in range(2):
                    pp = ps[3 + qk]
                    for j in range(8):
                        k.mm(pp[:], wq(wbuf, j, qk * 128, (qk + 1) * 128, 256), XC(j), j == 0, j == 7, [wk, "xT"], [pn(3 + qk)])
                    pr = pre[qk]
                    prk = "pre%d" % qk
                    ci = qk * 4 + h
                    k.cp("vector", pr[:, 0:3], qkcar[:, ci, :], ["qkcar"], [prk])
                    k.cp("scalar", pr[:, 3:515], pp[:], [pn(3 + qk)], [prk])
                    k.cp("vector", qkcar[:, ci, :], pr[:, 512:515], [prk], ["qkcar"])
                    wn = "cq_w" if qk == 0 else "ck_w"
                    bn = "cq_b" if qk == 0 else "ck_b"
                    acc = Tt[4 + qk]
                    ak = tn(4 + qk)
                    k.ts("vector", acc[:], pr[:, 0:512], pvc(wn, 0 * 4 + h), ALU.mult, [prk, "pv"], [ak])
                    for tap in range(1, 4):
                        k.stt(acc[:], pr[:, tap:tap + 512], pvc(wn, tap * 4 + h), acc[:], ALU.mult, ALU.add, [prk, "pv", ak], [ak])
                    k.act(acc[:], acc[:], AF.Silu, [ak, "pv"], [ak], bias=pvc(bn, h))
                    if qk == 0:
                        k.tt("vector", qt[:, h, :], acc[:], Tt[6][:], ALU.mult, [ak, tn(6)], ["qt%d" % h])
                    else:
                        k.stt(kt[:, h, :], acc[:], 128.0 ** -0.5, Tt[3][:], ALU.mult, ALU.mult, [ak, tn(3)], ["kt%d" % h])
            if blk == BLKS[-1]:
                dump("rows8", rows8[:], "T7")
                dump("EGs", EGs[:], "EGs3")
                dump("qt", qkt[:], "qt3")
                pass
                dump("T3", Tt[3][:], tn(3))
                dump("T4", Tt[4][:], tn(4))
                dump("T5", Tt[5][:], tn(5))
            wbuf, wk = load_piece(PIDX["mlv"][0], 8 * 512)
            for tt in range(4):
                pp = ps[tt % 2]
                for j in range(8):
                    k.mm(pp[:], xT[:, j, 1 + tt * 128:1 + (tt + 1) * 128], wq(wbuf, j, 0, 512, 512), j == 0, j == 7, [wk, "xT"], [pn(tt % 2)])
                k.cp("scalar", vaug[:, tt, :, 0:128], pp[:].rearrange("p (h n) -> p h n", h=4), [pn(tt % 2)], ["vaug"])
            if own:
                wbuf, wk = load_piece(PIDX["mlo"][0], 8 * 512)
                for tt in range(4):
                    pp = ps[2 + tt % 2]
                    for j in range(8):
                        k.mm(pp[:], xT[:, j, 1 + tt * 128:1 + (tt + 1) * 128], wq(wbuf, j, 0, 512, 512), j == 0, j == 7, [wk, "xT"], [pn(2 + tt % 2)])
                    k.act(sigo[:, tt, :], pp[:], AF.Sigmoid, [pn(2 + tt % 2)], ["sigo"])
            for tt in range(4):
                for h in range(4):
                    u = (tt * 4 + h) % 2
                    csl = slice(tt * 128, (tt + 1) * 128)
                    k.tr(pb[:, u * 128:(u + 1) * 128], kt[:, h, csl], ident[:], ["kt%d" % h, "ident"], ["pb"])
                    k.cp("scalar", ktok[u][:], pb[:, u * 128:(u + 1) * 128], ["pb"], ["ktok%d" % u])
                    k.mm(ps[4][:, u * 128:(u + 1) * 128], kt[:, h, csl], qt[:, h, csl], True, True, ["kt%d" % h, "qt%d" % h], ["ps4_%d" % u])
                    k.tt("vector", PTt[u][:], ps[4][:, u * 128:(u + 1) * 128], tri[:], ALU.mult, ["ps4_%d" % u, "tri"], ["PT%d" % u])
                    if own:
                        pN = ps[5][:, u * 256:u * 256 + 129]
                        pk = "ps5_%d" % u
                        k.mm(pN, qt[:, h, csl], CTb[:, h, :], True, False, ["qt%d" % h, "CTb%d" % h], [pk])
                        k.mm(pN, PTt[u][:], vaug[:, tt, h, :], False, True, ["PT%d" % u, "vaug"], [pk])
                        k.act(sm[:, 4:5], ps[5][:, u * 256 + 128:u * 256 + 129], AF.Abs, [pk], ["sm4"])
                        k.ts("vector", sm[:, 4:5], sm[:, 4:5], 1.0, ALU.max, ["sm4"], ["sm4"])
                        P.op("vector", lambda e: e.reciprocal(out=sm[:, 5:6], in_=sm[:, 4:5]), ["sm4"], ["sm5"])
                        k.ts("vector", hh_[u][:], ps[5][:, u * 256:u * 256 + 128], sm[:, 5:6], ALU.mult, [pk, "sm5"], ["hh%d" % u])
                        P.op("vector", lambda e, u=u: e.bn_stats(out=st6[:, 0:6], in_=hh_[u][:]), ["hh%d" % u], ["st6"])
                        P.op("vector", lambda e: e.bn_aggr(out=sm[:, 6:8], in_=st6[:, 0:6]), ["st6"], ["sm6"])
                        k.act(sm[:, 8:9], sm[:, 7:8], AF.Ln, ["sm6", "negs"], ["sm8"], bias=negs[:, 16:17])
                        k.act(sm[:, 8:9], sm[:, 8:9], AF.Exp, ["sm8"], ["sm8"], scale=-0.5)
                        k.ts("vector", hh_[u][:], hh_[u][:], sm[:, 6:7], ALU.subtract, ["hh%d" % u, "sm6", "sm8"], ["hh%d" % u], s2=sm[:, 8:9], op1=ALU.mult)
                        k.tt("vector", hh_[u][:], hh_[u][:], mlnw[:, h * 128:(h + 1) * 128], ALU.mult, ["hh%d" % u, "mlnw"], ["hh%d" % u])
                        k.tt("vector", ymlt[:, tt, h * 128:(h + 1) * 128], hh_[u][:], sigo[:, tt, h * 128:(h + 1) * 128], ALU.mult, ["hh%d" % u, "sigo"], ["sigo"])
                    pU = ps[6][:, u * 256:u * 256 + 129]
                    k.mm(pU, ktok[u][:], vaug[:, tt, h, :], True, True, ["ktok%d" % u, "vaug"], ["ps6_%d" % u])
                    sc = 1.0 if own else pvc("flag")
                    k.stt(tmpC[:], pU, sc, CT[:, h, :], ALU.mult, ALU.add, ["ps6_%d" % u, "CT%d" % h, "pv"], ["tmpC"])
                    k.ts("vector", CT[:, h, :], tmpC[:], EGs[:, h, tt:tt + 1], ALU.mult, ["tmpC", "EGs%d" % h], ["CT%d" % h])
                    k.cp("scalar", CTb[:, h, :], CT[:, h, :], ["CT%d" % h], ["CTb%d" % h])
            if blk == BLKS[-1]:
                dump("vaug", vaug[:], "vaug")
                dump("CT", CT[:], "CT")
                dump("ymlt", ymlt[:], "sigo")
                dump("sigo", sigo[:], "sigo")
            if own:
                for tt in range(4):
                    for wc in range(4):
                        k.tr(pb[:, 512 + wc * 128:512 + (wc + 1) * 128], ymlt[:, tt, wc * 128:(wc + 1) * 128], ident[:], ["sigo", "ident"], ["pbh"])
                    k.cp("scalar", ymlT[:, :, tt * 128:(tt + 1) * 128], pb[:, 512:1024].rearrange("p (j n) -> p j n", j=4), ["pbh"], ["ymlT"])
                if dbg:
                    ob = (blk - 4) * 512
                    for tt in range(4):
                        k.cp("vector", Tt[0][:], ymlt[:, tt, :], ["sigo"], [tn(0)])
                        P.dma("sync", G["dbg_yml"][ob + tt * 128:ob + (tt + 1) * 128, :], Tt[0][:], reads=[tn(0)])

        if 'C' in STAGES:
            wbuf, wk = load_piece(PIDX["lo"][0], 8 * 512)
            for (pi_, c0) in [(0, 0), (1, 128)]:
                for j in range(8):
                    k.mm(ps[pi_][:], wq(wbuf, j, c0, c0 + 128, 512), XC(j), j == 0, False, [wk, "xT"], [pn(pi_)])
                for j in range(8):
                    k.mm(ps[pi_][:], wq(wbuf, j, 256 + c0, 256 + c0 + 128, 512), XP(j), False, j == 7, [wk, "xT"], [pn(pi_)])
            k.act(twa[0:64, :], ps[0][0:64, :], AF.Tanh, [pn(0)], ["twa"])
            k.cp("vector", twa[64:128, :], ps[0][64:128, :], [pn(0)], ["twa"])
            k.act(sgz[:], ps[1][:], AF.Sigmoid, [pn(1)], ["sgz"])
            hmb = hm[:].unsqueeze(1).unsqueeze(3).to_broadcast([128, 8, 2, 64])
            bc4 = lambda t_: t_[:].rearrange("p (c s) -> p c s", s=64).unsqueeze(2).to_broadcast([128, 8, 2, 64])
            Tr, Tk, Tew, Tcs, Twi, Twv, Twe, Ta, T3, T4, T5, Tkp = [Tt[i] for i in range(12)]
            nr, nk, new, ncs, nwi, nwv, nwe, na, n3, n4, n5, nkp = [tn(i) for i in range(12)]

            def hpbuf(hp):
                s2 = hp % 2
                return (s2, ARbd[s2], BKbd[s2], VbT[s2], WLs[s2], "ARbd%d" % s2, "BKbd%d" % s2, "VbT%d" % s2, "WLs%d" % s2)

            def prep(hp):
                s2, AR, BK, VB, WL, nAR, nBK, nVB, nWL = hpbuf(hp)
                Tg, ng = TgT[s2], "TgT%d" % s2
                vT_, nvT = vTb2[s2], "vTb%d" % s2
                pb_, npb = pbon2[s2], "pbon%d" % s2
                wA, wAk = load_piece(PIDX["rwA%d" % hp][0], 8 * 384)
                wB, wBk = load_piece(PIDX["rwB%d" % hp][0], 8 * 384)
                for ci in range(3):
                    for j in range(8):
                        k.mm(ps[ci][:], wq(wA, j, ci * 128, (ci + 1) * 128, 384), XC(j), j == 0, False, [wAk, "xT"], [pn(ci)])
                    for j in range(8):
                        k.mm(ps[ci][:], wq(wB, j, ci * 128, (ci + 1) * 128, 384), XP(j), False, j == 7, [wBk, "xT"], [pn(ci)])
                hs = slice(hp * 128, (hp + 1) * 128)
                k.mm(ps[5][:], loraw[0:64, hs], twa[0:64, :], True, True, ["loraw", "twa"], [pn(5)])
                k.mm(ps[6][:], loraw[64:128, hs], twa[64:128, :], True, True, ["loraw", "twa"], [pn(6)])
                k.cp("scalar", Tr[:], ps[0][:], [pn(0)], [nr])
                k.cp("scalar", Tk[:], ps[1][:], [pn(1)], [nk])
                k.cp("vector", vT_[:], ps[2][:], [pn(2)], [nvT])
                k.act(T5[:], ps[5][:], AF.Exp, [pn(5), "negs"], [n5], bias=negs[:, hp:hp + 1], scale=-1.0)
                k.act(T5[:], T5[:], AF.Ln, [n5, "negs"], [n5], bias=ones1)
                k.act(Tew[:], T5[:], AF.Exp, [n5, "negs"], [new], bias=negs[:, 13:14], scale=-1.0)
                P.op("vector", lambda e: e.tensor_tensor_scan(out=Tt[3][:], data0=rm64[:], data1=Tt[2][:], initial=0.0, op0=ALU.mult, op1=ALU.add), [new, "rm64"], [ncs])
                k.act(Twi[:], Tcs[:], AF.Exp, [ncs], [nwi], scale=-1.0)
                k.act(Twv[:], Tcs[:], AF.Exp, [ncs], [nwv])
                k.tt("vector", T5[:], Tcs[:], Tew[:], ALU.subtract, [ncs, new], [n5])
                k.act(Twe[:], T5[:], AF.Exp, [n5], [nwe], scale=-1.0)
                k.act(Ta[:], ps[6][:], AF.Sigmoid, [pn(6), "pv"], [na], bias=pvc("a0", hp))
                if own:
                    k.mm(ps[0][:], gupw[:, hs], sgz[:], True, True, ["gupw", "sgz"], [pn(0)])
                    k.cp("scalar", Tg[:], ps[0][:], [pn(0)], [ng])
                k.ts("vector", T3[:], Tk[:], pvc("k_k", hp), ALU.mult, [nk, "pv"], [n3])
                k.tt("vector", T4[:], T3[:], T3[:], ALU.mult, [n3], [n4])
                k.mm(ps[1][:], bdf[:], T4[:], True, True, ["bdf", n4], [pn(1)])
                k.ts("vector", T4[:], ps[1][:], 1e-18, ALU.max, [pn(1)], [n4])
                k.act(T4[:], T4[:], AF.Ln, [n4], [n4])
                k.act(T4[:], T4[:], AF.Exp, [n4], [n4], scale=-0.5)
                k.tt("vector", T3[:], T3[:], T4[:], ALU.mult, [n3, n4], [n3])
                k.ts("vector", T4[:], Ta[:], pvc("k_a", hp), ALU.mult, [na, "pv", "negs"], [n4], s2=negs[:, 4 + hp:5 + hp], op1=ALU.add)
                k.tt("vector", Tkp[:], Tk[:], T4[:], ALU.mult, [nk, n4], [nkp])
                k.tt("vector", T4[:], T3[:], Ta[:], ALU.mult, [n3, na], [n4])
                k.stt(T5[:], T3[:], -1.0, Twe[:], ALU.mult, ALU.mult, [n3, nwe], [n5])
                k.tt("vector", AR[:, :, 0:128].rearrange("p c (h s) -> p c h s", h=2), bc4(T5), hmb, ALU.mult, [n5, "hm"], [nAR])
                k.tt("vector", AR[:, :, 128:192], Tr[:].rearrange("p (c s) -> p c s", s=64), Twi[:].rearrange("p (c s) -> p c s", s=64), ALU.mult, [nr, nwi], [nAR])
                k.tt("vector", T4[:], T4[:], Twv[:], ALU.mult, [n4, nwv], [n4])
                k.tt("gpsimd", BK[:, :, 0:128].rearrange("p c (h s) -> p c h s", h=2), bc4(T4), hmb, ALU.mult, [n4, "hm"], [nBK])
                k.tt("vector", T5[:], Tkp[:], Twv[:], ALU.mult, [nkp, nwv], [n5])
                k.tt("gpsimd", BK[:, :, 128:256].rearrange("p c (h s) -> p c h s", h=2), bc4(T5), hmb, ALU.mult, [n5, "hm"], [nBK])
                k.tt("gpsimd", VB[:].rearrange("p c (h s) -> p c h s", h=2), vT_[:].rearrange("p (c s) -> p c s", s=64).unsqueeze(2).to_broadcast([128, 8, 2, 64]), hmb, ALU.mult, [nvT, "hm"], [nVB])
                k.cp("vector", WL[:], Twi[:].rearrange("p (c s) -> p c s", s=64)[:, :, 63], [nwi], [nWL])
                if own:
                    k.stt(pb_[:], Tr[:], pvc("r_k", hp), Tkp[:], ALU.mult, ALU.mult, [nr, nkp, "pv"], [npb])

            SQB = [0, 1, 2]

            def steps_gen(hp):
                s2, AR, BK, VB, WL, nAR, nBK, nVB, nWL = hpbuf(hp)
                XM_, TT_, M2_, TOK_ = XM[s2], TTb[s2], M2[s2], TOK[s2]
                kx = lambda nm, c: "%s%d_%d" % (nm, s2, c)
                for c in range(NCH):
                    bA, bB = (0, 1) if c % 2 == 0 else (2, 5)
                    A_bd = AR[:, c, 0:128]
                    B_bd = BK[:, c, 0:128]
                    K_bd = BK[:, c, 128:256]
                    k.mm(ps[bA][:, 0:192], B_bd, AR[:, c, :], True, True, [nBK, nAR], [pn(bA)])
                    k.mm(ps[bA][:, 256:384], A_bd, B_bd, True, True, [nBK, nAR], [pn(bA)])
                    k.mm(ps[bB][:, 0:192], K_bd, AR[:, c, :], True, True, [nBK, nAR], [pn(bB)])
                    yield
                    k.tt("vector", XM_[c][:], ps[bA][:, 0:192], MU192[:], ALU.mult, [pn(bA), "MU192"], [kx("XM", c)])
                    k.tt("vector", XTb[c][:], ps[bA][:, 256:384], MLs[:], ALU.mult, [pn(bA), "MLs"], ["XTb%d" % c])
                    yield
                    k.tt("vector", M2_[c][:], ps[bB][:, 0:192], MU192[:], ALU.mult, [pn(bB), "MU192"], [kx("M2", c)])
                    k.tt("gpsimd", TT_[c][:], XM_[c][:, 0:128], ident[:], ALU.add, [kx("XM", c), "ident"], [kx("TT", c)])
                    yield
                for lvl in range(1, 6):
                    for c in range(NCH):
                        if lvl == 1:
                            cur, curT, kcur = XM_[c][:, 0:128], XTb[c][:], [kx("XM", c), "XTb%d" % c]
                        else:
                            src = PA[c] if lvl % 2 == 0 else PB[c]
                            cur, curT, kcur = src[:, 128:256], src[:, 0:128], [("PA%d" if lvl % 2 == 0 else "PB%d") % c]
                        dst = PA[c] if lvl % 2 == 1 else PB[c]
                        kdst = ("PA%d" if lvl % 2 == 1 else "PB%d") % c
                        bk = SQB[c % 3]
                        k.mm(ps[bk][:, 0:128], cur, curT, True, True, kcur, [pn(bk)])
                        if lvl < 5:
                            k.mm(ps[bk][:, 128:256], curT, cur, True, True, kcur, [pn(bk)])
                        wdt = 256 if lvl < 5 else 128
                        k.cp("scalar" if c % 2 else "vector", dst[:, 0:wdt], ps[bk][:, 0:wdt], [pn(bk)], [kdst])
                        yield
                    for c in range(NCH):
                        dst = PA[c] if lvl % 2 == 1 else PB[c]
                        kdst = ("PA%d" if lvl % 2 == 1 else "PB%d") % c
                        ba = 5 + c % 2
                        k.mm(ps[ba][:, 0:128], dst[:, 0:128], TT_[c][:], True, True, [kdst, kx("TT", c)], [pn(ba)])
                        k.tt("vector", TT_[c][:], ps[ba][:, 0:128], TT_[c][:], ALU.add, [pn(ba), kx("TT", c)], [kx("TT", c)])
                        yield
                for c in range(NCH):
                    k.tr(pb[:, 0:128], BK[:, c, 0:128], ident[:], [nBK, "ident"], ["pb"])
                    k.tr(pb[:, 128:256], BK[:, c, 128:256], ident[:], [nBK, "ident"], ["pb"])
                    k.tr(pb[:, 256:384], VB[:, c, :], ident[:], [nVB, "ident"], ["pb"])
                    k.cp("scalar", TOK_[c][:], pb[:, 0:384].rearrange("p (a n) -> p a n", a=3), ["pb"], [kx("TOK", c)])
                    yield

            def chain_gen(hp):
                s2, AR, BK, VB, WL, nAR, nBK, nVB, nWL = hpbuf(hp)
                XM_, TT_, M2_, TOK_ = XM[s2], TTb[s2], M2[s2], TOK[s2]
                kx = lambda nm, c: "%s%d_%d" % (nm, s2, c)
                for c in range(NCH):
                    d = c % 2
                    A_bd = AR[:, c, 0:128]
                    R_st = AR[:, c, 128:192]
                    Btok, Ktok_, Vbd = TOK_[c][:, 0, :], TOK_[c][:, 1, :], TOK_[c][:, 2, :]
                    k.mm(ps[3][:, 0:128], A_bd, STb[:, hp, :], True, False, [nAR, "STb%d" % hp], ["ps3a"])
                    k.mm(ps[3][:, 0:128], M2_[c][:, 0:128], Vbd, False, True, [kx("M2", c), kx("TOK", c)], ["ps3a"])
                    yield
                    k.cp("scalar", RHSb[d][:], ps[3][:, 0:128], ["ps3a"], ["RHSb%d" % d])
                    yield
                    k.mm(ps[3][:, 128:256], TT_[c][:], RHSb[d][:], True, True, [kx("TT", c), "RHSb%d" % d], ["ps3b"])
                    yield
                    k.cp("vector", Usb[d][:], ps[3][:, 128:256], ["ps3b"], ["Usb%d" % d])
                    yield
                    k.mm(ps[3][:, 256:384], Btok, Usb[d][:], True, False, [kx("TOK", c), "Usb%d" % d], ["ps3c"])
                    k.mm(ps[3][:, 256:384], Ktok_, Vbd, False, True, [kx("TOK", c)], ["ps3c"])
                    if own:
                        pY = ps[4][:, c * 64:(c + 1) * 64]
                        k.mm(pY, STb[:, hp, :], R_st, True, False, ["STb%d" % hp, nAR], [pn(4)])
                        k.mm(pY, Usb[d][:], XM_[c][:, 128:192], False, False, ["Usb%d" % d, kx("XM", c)], [pn(4)])
                        k.mm(pY, Vbd, M2_[c][:, 128:192], False, True, [kx("TOK", c), kx("M2", c)], [pn(4)])
                    yield
                    k.tt("vector", tmpS[:], ps[3][:, 256:384], ST[:, hp, :], ALU.add, ["ps3c", "ST%d" % hp], ["tmpS"])
                    yield
                    k.ts("vector", STb[:, hp, :], tmpS[:], WL[:, c:c + 1], ALU.mult, ["tmpS", nWL], ["STb%d" % hp])
                    k.ts("gpsimd", ST[:, hp, :], tmpS[:], WL[:, c:c + 1], ALU.mult, ["tmpS", nWL], ["ST%d" % hp])
                    yield

            def gn(hp):
                s2 = hp % 2
                Tg, ng = TgT[s2], "TgT%d" % s2
                vT_, nvT = vTb2[s2], "vTb%d" % s2
                pb_, npb = pbon2[s2], "pbon%d" % s2
                Y, Y2 = Tt[12], Tt[13]
                nY, nY2 = tn(12), tn(13)
                k.cp("scalar", Y[:], ps[4][:], [pn(4)], [nY])
                k.mm(ps[5][:], bo64[:], Y[:], True, True, ["bo64", nY], [pn(5)])
                k.tt("vector", Y[:], Y[:], ps[5][:], ALU.subtract, [nY, pn(5)], [nY])
                k.tt("vector", Y2[:], Y[:], Y[:], ALU.mult, [nY], [nY2])
                k.mm(ps[6][:], bo64[:], Y2[:], True, True, ["bo64", nY2], [pn(6)])
                k.act(Y2[:], ps[6][:], AF.Ln, [pn(6), "negs"], [nY2], bias=negs[:, 17:18])
                k.act(Y2[:], Y2[:], AF.Exp, [nY2], [nY2], scale=-0.5)
                k.tt("vector", Y[:], Y[:], Y2[:], ALU.mult, [nY, nY2], [nY])
                k.ts("vector", Y[:], Y[:], pvc("gn_w", hp), ALU.mult, [nY, "pv"], [nY], s2=pvc("gn_b", hp), op1=ALU.add)
                k.mm(ps[5][:], bdm[:], pb_[:], True, True, ["bdm", npb], [pn(5)])
                k.tt("vector", Y2[:], ps[5][:], vT_[:], ALU.mult, [pn(5), nvT], [nY2])
                k.tt("vector", Y[:], Y[:], Y2[:], ALU.add, [nY, nY2], [nY])
                k.tt("vector", yrwT[:, hp, :], Y[:], Tg[:], ALU.mult, [nY, ng], ["yrwT"])
                if dbg:
                    k.tt("vector", Y2[:], Y[:], Tg[:], ALU.mult, [nY, ng], [nY2])
                    ob = (blk - 4) * 512
                    P.dma("sync", G["dbg_yrw"][hp * 128:(hp + 1) * 128, ob:ob + 512], Y2[:], reads=[nY2])

            def drain(g):
                for _ in g:
                    pass

            def merge(ga, gb, ratio=3):
                doneb = False
                for _ in ga:
                    if not doneb:
                        for _r in range(ratio):
                            try:
                                next(gb)
                            except StopIteration:
                                doneb = True
                                break
                if not doneb:
                    drain(gb)

            prep(0)
            drain(steps_gen(0))
            for hp in range(NHP):
                if hp + 1 < NHP:
                    prep(hp + 1)
                    merge(chain_gen(hp), steps_gen(hp + 1))
                else:
                    drain(chain_gen(hp))
                if own:
                    gn(hp)

        if own and 'D' in STAGES:
            for dc in range(8):
                wg, wgk = load_piece(PIDX["gate%d" % dc][0], 8 * 256, extra=[(PI_PRW, dc * 512, 512, 2048), (PI_PML, dc * 512, 512, 2560)])
                o = 0 if dc % 2 == 0 else 3
                wP3 = wg[:, 2048:2560].rearrange("p (j n) -> p j n", j=4)
                wM3 = wg[:, 2560:3072].rearrange("p (j n) -> p j n", j=4)
                for wc in range(4):
                    k.mm(ps[o][:], wP3[:, wc, :], yrwT[:, wc, :], wc == 0, wc == 3, [wgk, "yrwT"], [pn(o)])
                for wc in range(4):
                    k.mm(ps[o + 1][:], wM3[:, wc, :], ymlT[:, wc, :], wc == 0, wc == 3, [wgk, "ymlT"], [pn(o + 1)])
                Ga, Gb = Tt[0], Tt[1]
                for gi in range(2):
                    for j in range(8):
                        k.mm(ps[o + 2][:], wq(wg, j, gi * 128, (gi + 1) * 128, 256), XC(j), j == 0, j == 7, [wgk, "xT"], [pn(o + 2)])
                    k.act([Ga, Gb][gi][:], ps[o + 2][:], AF.Sigmoid, [pn(o + 2), "pv"], [tn(gi)], bias=pvc("gate_b", gi * 8 + dc))
                k.tt("vector", Ga[:], Ga[:], ps[o][:], ALU.mult, [tn(0), pn(o)], [tn(0)])
                k.tt("vector", Gb[:], Gb[:], ps[o + 1][:], ALU.mult, [tn(1), pn(o + 1)], [tn(1)])
                k.tt("vector", mergedT[:, dc, :], Ga[:], Gb[:], ALU.add, [tn(0), tn(1)], MKEYS)
            w0_, w0k = load_piece(PI_WO0, 4096)
            w1_, w1k = load_piece(PI_WO1, 4096)
            for tt in range(4):
                lt = blk * 4 + tt
                ot = (blk - 4) * 4 + tt
                xs_ = xt[lt % 2]
                xk = "xt0"
                P.dma("sync", xs_[:], xs[lt * 128:(lt + 1) * 128, :], writes=[xk])
                for half, (wo, wok) in enumerate([(w0_, w0k), (w1_, w1k)]):
                    pp = ps[half + 2 * (tt % 2)]
                    ppk = pn(half + 2 * (tt % 2))
                    for dc in range(8):
                        k.mm(pp[:], mergedT[:, dc, tt * 128:(tt + 1) * 128], wq(wo, dc, 0, 512, 512), dc == 0, dc == 7, MKEYS + [wok], [ppk])
                    k.tt("vector", xs_[:, half * 512:(half + 1) * 512], xs_[:, half * 512:(half + 1) * 512], pp[:], ALU.add, [xk, ppk], [xk])
                P.dma("sync", x1_d[ot * 128:(ot + 1) * 128, :], xs_[:], reads=[xk], writes=["x1d%d" % ot])
                if dbg:
                    P.dma("sync", G["dbg_x1"][ot * 128:(ot + 1) * 128, :], xs_[:], reads=[xk])
        k.cp("vector", xT[:, :, 0:1], xT[:, :, 512:513], ["xT"], ["xT"])


def _peer(nc, P, k, st0, G):
    pv, ident, identf, pvc = G["pv"], G["ident"], G["identf"], G["pvc"]
    x1_d, G_d, out_d = G["x1_d"], G["G_d"], G["out_d"]
    g2col = lambda j: pv[:, PV["g2"] + j:PV["g2"] + j + 1]
    T0 = lambda n, s, d: st0.enter_context(nc.sbuf_tensor("sb_" + n, s, d))
    hn2T = T0("hn2T", [128, 8, 2048], BF16)
    eps6 = T0("eps6", [128, 1], F32)
    k.memset("vector", eps6[:], 1e-6, ["eps6"])
    P.alias.clear()
    P.excl.clear()
    P.excl.update(["q%d" % i for i in range(8)])
    NTI = len(PTILES)

    stA = ExitStack()
    abgT = stA.enter_context(nc.sbuf_tensor("sb_abgT", [128, 3, 2048], F32))
    with ExitStack() as st:
        T = lambda n, s, d: st.enter_context(nc.sbuf_tensor("sb_" + n, s, d))
        PS = lambda n, s, d: st.enter_context(nc.psum_tensor("pp_" + n, s, d))
        Wq = T("Wq", [128, 8, 2048], BF16)
        wst = T("wstq", [128, 8, 512], F32)
        skT = T("skT", [128, 16, 128], BF16)
        qT = T("qT", [128, 16, 512], BF16)
        s_all = T("s_all", [128, 16, 128], F32)
        work = T("work", [128, 128], F32)
        tops = T("tops", [128, 16, 16], F32)
        idx = T("idx", [128, 16, 16], U32)
        idxf = T("idxf", [128, 16, 16], F32)
        cand = T("cand", [128, 8, 256], F32)
        workc = T("workc", [128, 256], F32)
        best = T("best", [128, 8, 16], F32)
        pos = T("pos", [128, 8, 16], U32)
        pq = T("pq", [128, 2, 128], U32)
        pqf = T("pqf", [128, 2, 128], F32)
        eg = T("eg", [128, 8, 16], F32)
        zz = T("zz", [128, 16], F32)
        eq = T("eq", [128, 128, 16], F32)
        abg = T("abg", [128, 3, 128], F32)
        iota16 = T("iota16", [128, 16], F32)
        io32 = T("io32", [128, 16], I32)
        xt = [T("pxt%d" % i, [128, 1024], F32) for i in range(2)]
        xnb = T("pxnb", [128, 1024], BF16)
        junk = T("pjunk", [128, 1024], BF16)
        sm = T("psm", [128, 8], F32)
        pb = PS("qb", [128, 1024], BF16)
        ps = [PS("qs%d" % i, [128, 512], F32) for i in range(7)]
        pn = lambda i: "q%d" % i
        P.op("gpsimd", lambda e: e.iota(io32[:], pattern=[[1, 16]], base=0, channel_multiplier=0), (), ["io32"])
        k.cp("vector", iota16[:], io32[:], ["io32"], ["iota16"])
        wqv = G["wq_d"].rearrange("(j p) n -> p j n", p=128)
        g2b = pv[:, PV["g2"]:PV["g2"] + 8].unsqueeze(2).to_broadcast([128, 8, 512])
        for pc in range(4):
            P.dma("sync", wst[:], wqv[:, :, pc * 512:(pc + 1) * 512], writes=["wstq"])
            k.tt("vector", Wq[:, :, pc * 512:(pc + 1) * 512], wst[:], g2b, ALU.mult, ["wstq", "pv"], ["Wq"])
        P.dma("sync", wst[:].rearrange("p j n -> p (j n)")[:, 0:2048], G["skT_d"], writes=["wstq"])
        k.cp("vector", skT[:].rearrange("p g n -> p (g n)"), wst[:].rearrange("p j n -> p (j n)")[:, 0:2048], ["wstq"], ["skT"])
        for sti in range((NTI + 3) // 4):
            tiles = PTILES[sti * 4:(sti + 1) * 4]
            for tt, ti in enumerate(tiles):
                xs_ = xt[ti % 2]
                xk = "pxt%d" % (ti % 2)
                P.dma("sync", xs_[:], x1_d[ti * 128:(ti + 1) * 128, :], reads=["x1d%d" % ti], writes=[xk])
                k.memset("vector", sm[:, 0:1], 0.0, ["psm0"])
                P.op("scalar", lambda e, xs_=xs_: e.activation(out=junk[:], in_=xs_[:], func=AF.Square, scale=1.0 / 32.0, accum_out=sm[:, 0:1]), [xk, "psm0"], ["pjunk", "psm0"])
                k.act(sm[:, 1:2], sm[:, 0:1], AF.Ln, ["psm0", "eps6"], ["psm1"], bias=eps6[:])
                k.act(sm[:, 1:2], sm[:, 1:2], AF.Exp, ["psm1"], ["psm1"], scale=-0.5)
                k.ts("vector", xnb[:], xs_[:], sm[:, 1:2], ALU.mult, [xk, "psm1"], ["pxnb"])
                for j in range(8):
                    k.tr(pb[:, j * 128:(j + 1) * 128], xnb[:, j * 128:(j + 1) * 128], ident[:], ["pxnb", "ident"], ["q7"])
                k.cp("scalar", hn2T[:, :, ti * 128:(ti + 1) * 128], pb[:].rearrange("p (j n) -> p j n", j=8), ["q7"], ["hn2T"])
            nt = len(tiles) * 128
            c0 = tiles[0] * 128
            for g in range(16):
                pp = ps[g % 2]
                for j in range(8):
                    k.mm(pp[:, 0:nt], Wq[:, j, g * 128:(g + 1) * 128], hn2T[:, j, c0:c0 + nt], j == 0, j == 7, ["Wq", "hn2T"], [pn(g % 2)])
                k.cp("scalar" if g % 2 else "vector", qT[:, g, 0:nt], pp[:, 0:nt], [pn(g % 2)], ["qT"])
            for tt, ti in enumerate(tiles):
                for gg in range(4):
                    pp = ps[2 + gg % 2]
                    for g4 in range(4):
                        g = gg * 4 + g4
                        k.mm(pp[:, g4 * 128:(g4 + 1) * 128], qT[:, g, tt * 128:(tt + 1) * 128], skT[:, g, :], True, True, ["qT", "skT"], [pn(2 + gg % 2)])
                    k.cp("scalar", s_all[:, gg * 4:(gg + 1) * 4, :], pp[:].rearrange("p (g n) -> p g n", g=4), [pn(2 + gg % 2)], ["s_all"])
                V = "vector"
                for g in range(16):
                    P.op(V, lambda e, g=g: e.max(out=tops[:, g, 0:8], in_=s_all[:, g, :]), ["s_all"], ["tops"])
                    P.op(V, lambda e, g=g: e.max_index(out=idx[:, g, 0:8], in_max=tops[:, g, 0:8], in_values=s_all[:, g, :]), ["s_all", "tops"], ["idx"])
                    P.op(V, lambda e, g=g: e.match_replace(out=work[:], in_to_replace=tops[:, g, 0:8], in_values=s_all[:, g, :], imm_value=-1e30), ["s_all", "tops"], ["work"])
                    P.op(V, lambda e, g=g: e.max(out=tops[:, g, 8:16], in_=work[:]), ["work"], ["tops"])
                    P.op(V, lambda e, g=g: e.max_index(out=idx[:, g, 8:16], in_max=tops[:, g, 8:16], in_values=work[:]), ["work", "tops"], ["idx"])
                t4 = tops[:].rearrange("p (h q) i -> p h q i", q=2)
                k.tt(V, cand[:].rearrange("p h (i j) -> p h i j", j=16), t4[:, :, 0, :].unsqueeze(3).to_broadcast([128, 8, 16, 16]),
                     t4[:, :, 1, :].unsqueeze(2).to_broadcast([128, 8, 16, 16]), ALU.add, ["tops"], ["cand"])
                for h in range(8):
                    P.op(V, lambda e, h=h: e.max(out=best[:, h, 0:8], in_=cand[:, h, :]), ["cand"], ["best"])
                    P.op(V, lambda e, h=h: e.max_index(out=pos[:, h, 0:8], in_max=best[:, h, 0:8], in_values=cand[:, h, :]), ["cand", "best"], ["pos"])
                    P.op(V, lambda e, h=h: e.match_replace(out=workc[:], in_to_replace=best[:, h, 0:8], in_values=cand[:, h, :], imm_value=-1e30), ["cand", "best"], ["workc"])
                    P.op(V, lambda e, h=h: e.max(out=best[:, h, 8:16], in_=workc[:]), ["workc"], ["best"])
                    P.op(V, lambda e, h=h: e.max_index(out=pos[:, h, 8:16], in_max=best[:, h, 8:16], in_values=workc[:]), ["workc", "best"], ["pos"])
                k.tt(V, eg[:], best[:], best[:, :, 0:1].to_broadcast([128, 8, 16]), ALU.subtract, ["best"], ["eg"])
                k.act(eg[:], eg[:], AF.Exp, ["eg"], ["eg"])
                P.op(V, lambda e: e.tensor_reduce(out=zz[:, 0:8], in_=eg[:], axis=AX.X, op=ALU.add), ["eg"], ["zz"])
                P.op(V, lambda e: e.reciprocal(out=zz[:, 8:16], in_=zz[:, 0:8]), ["zz"], ["zz"])
                k.tt(V, abg[:, 2, :].rearrange("p (h n) -> p h n", h=8), eg[:], zz[:, 8:16].unsqueeze(2).to_broadcast([128, 8, 16]), ALU.mult, ["eg", "zz"], ["abg"])
                posf = pos[:].rearrange("p h n -> p (h n)")
                P.op(V, lambda e: e.tensor_single_scalar(out=pq[:, 0, :], in_=posf, scalar=4, op=ALU.logical_shift_right), ["pos"], ["pq"])
                P.op(V, lambda e: e.tensor_single_scalar(out=pq[:, 1, :], in_=posf, scalar=15, op=ALU.bitwise_and), ["pos"], ["pq"])
                k.cp(V, pqf[:], pq[:], ["pq"], ["pqf"])
                k.cp(V, idxf[:], idx[:], ["idx"], ["idxf"])
                i4 = idxf[:].rearrange("p (h q) i -> p h q i", q=2)
                for w_ in range(2):
                    k.tt(V, eq[:].rearrange("p (h n) i -> p h n i", h=8), iota16[:].unsqueeze(1).unsqueeze(1).to_broadcast([128, 8, 16, 16]),
                         pqf[:, w_, :].rearrange("p (h n) -> p h n", h=8).unsqueeze(3).to_broadcast([128, 8, 16, 16]), ALU.is_equal, ["iota16", "pqf"], ["eq"])
                    k.tt(V, eq[:].rearrange("p (h n) i -> p h n i", h=8), eq[:].rearrange("p (h n) i -> p h n i", h=8),
                         i4[:, :, w_, :].unsqueeze(2).to_broadcast([128, 8, 16, 16]), ALU.mult, ["eq", "idxf"], ["eq"])
                    P.op(V, lambda e, w_=w_: e.tensor_reduce(out=abg[:, w_, :], in_=eq[:], axis=AX.X, op=ALU.add), ["eq"], ["abg"])
                for w_ in range(3):
                    P.op("tensor", lambda e, w_=w_: e.transpose(out=ps[4][:, w_ * 128:(w_ + 1) * 128], in_=abg[:, w_, :], identity=identf[:]), ["abg", "identf"], [pn(4)])
                k.cp("scalar", abgT[:, :, ti * 128:(ti + 1) * 128], ps[4][:, 0:384].rearrange("p (w n) -> p w n", w=3), [pn(4)], ["abgT"])
        if G["dbg"]:
            dd = nc.dram_tensor("dbg_abgT", [128, 3, 2048], F32, kind="ExternalOutput").ap()
            P.dma("sync", dd, abgT[:], reads=["abgT"])
        with nc.Block() as block:
            P.emit(block)

    with ExitStack() as st:
        T = lambda n, s, d: st.enter_context(nc.sbuf_tensor("sb_" + n, s, d))
        PS = lambda n, s, d: st.enter_context(nc.psum_tensor("pp_" + n, s, d))
        iotak = T("iotak", [128, 128], F32)
        iok32 = T("iok32", [128, 128], I32)
        OA = [T("OA%d" % i, [128, 64, 128], BF16) for i in range(2)]
        WB = [T("WB%d" % i, [128, 64, 128], BF16) for i in range(2)]
        Gs = [T("Gs%d" % i, [128, 128, 128], BF16) for i in range(2)]
        ps = [PS("rs%d" % i, [128, 512], F32) for i in range(8)]
        pn = lambda i: "q%d" % i
        P.op("gpsimd", lambda e: e.iota(iok32[:], pattern=[[1, 128]], base=0, channel_multiplier=0), (), ["iok32"])
        k.cp("vector", iotak[:], iok32[:], ["iok32"], ["iotak"])
        ikb = iotak[:].unsqueeze(1).to_broadcast([128, 64, 128])
        nev = 0
        P.children["Gd"] = ["Gd%d_%d" % (ti, cq) for ti in range(16) for cq in range(4)]
        halves = [(ti, half) for ti in PTILES for half in range(2)]

        def build(ix, part):
            ti, half = halves[ix]
            hb = ix % 2
            t0 = ti * 128 + half * 64
            bc = lambda w_: abgT[:, w_, t0:t0 + 64].unsqueeze(2).to_broadcast([128, 64, 128])
            if part == 0:
                k.tt("vector", OA[hb][:], ikb, bc(0), ALU.is_equal, ["iotak", "abgT"], ["OA%d" % hb])
            else:
                k.tt("vector", WB[hb][:], ikb, bc(1), ALU.is_equal, ["iotak", "abgT"], ["WB%d" % hb])
                k.tt("gpsimd", WB[hb][:], WB[hb][:], bc(2), ALU.mult, ["WB%d" % hb, "abgT"], ["WB%d" % hb])

        build(0, 0)
        build(0, 1)
        for ix, (ti, half) in enumerate(halves):
            hb = ix % 2
            gsb = Gs[ti % 2]
            gk = "Gs%d" % (ti % 2)
            for tq in range(16):
                if ix + 1 < len(halves) and tq in (2, 9):
                    build(ix + 1, 0 if tq == 2 else 1)
                b_ = nev % 8
                nev += 1
                for t4 in range(4):
                    t = tq * 4 + t4
                    k.mm(ps[b_][:, t4 * 128:(t4 + 1) * 128], OA[hb][:, t, :], WB[hb][:, t, :], True, True, ["OA%d" % hb, "WB%d" % hb], [pn(b_)])
                tl = half * 64 + tq * 4
                k.cp("vector" if tq % 3 == 2 else "scalar", gsb[:, :, tl:tl + 4].rearrange("p c t -> p t c"), ps[b_][:].rearrange("p (t c) -> p t c", t=4), [pn(b_)], [gk])
            if half == 1:
                for cq in range(4):
                    P.dma("sync", G_d[cq * 32:(cq + 1) * 32, :, ti * 128:(ti + 1) * 128].rearrange("c k t -> k c t"), gsb[:, cq * 32:(cq + 1) * 32, :], reads=[gk], writes=["Gd%d_%d" % (ti, cq)])
        with nc.Block() as block:
            P.emit(block)

    stA.close()
    with ExitStack() as st:
        T = lambda n, s, d: st.enter_context(nc.sbuf_tensor("sb_" + n, s, d))
        PS = lambda n, s, d: st.enter_context(nc.psum_tensor("pp_" + n, s, d))
        GS = 2
        acc = T("acc", [128, 16, 1024], F32)
        fgb = T("fgb", [128, 1024], F32)
        ust = [T("ust%d" % i, [128, GS, 1024], F32) for i in range(2)]
        vst = [T("vst%d" % i, [128, GS, 1024], F32) for i in range(2)]
        ub = [T("ub%d" % i, [128, GS, 1024], BF16) for i in range(2)]
        vb = [T("vb%d" % i, [128, GS, 1024], BF16) for i in range(2)]
        gb = [T("gb%d" % i, [128, GS, 2048], BF16) for i in range(2)]
        gl = [T("gl%d" % i, [128, 512], BF16) for i in range(2)]
        amq = [T("amq%d" % i, [128, GS, 512], BF16) for i in range(2)]
        sm = T("fsm", [128, 8], F32)
        junk = T("fjunk", [128, 1024], BF16)
        pS = [PS("bS%d" % i, [128, 512], F32) for i in range(2)]
        pA = [PS("bA%d" % i, [128, 512], F32) for i in range(6)]
        P.dma("sync", fgb[:], G["fg_d"].partition_broadcast(128), writes=["fgb"])
        for ti in PTILES:
            P.dma("sync", acc[:, ti, :], x1_d[ti * 128:(ti + 1) * 128, :], reads=["x1d%d" % ti], writes=["acc%d" % ti])
        g2b4 = pv[:, PV["g2"]:PV["g2"] + 8].unsqueeze(1).unsqueeze(3).to_broadcast([128, GS, 8, 128])
        quads = [PTILES[i:i + 4] for i in range(0, len(PTILES), 4)]
        steps = [(grp, q) for grp in range(NGRP) for q in range(len(quads))]
        cnt = {"set": 0, "S": 0}

        def loads(grp):
            s = grp % 2
            c0 = grp * GS
            for i in range(GS):
                P.dma("sync", ust[s][:, i, :], G["uT_d"][c0 + i], writes=["ust%d" % s])
                P.dma("sync", vst[s][:, i, :], G["vp_d"][c0 + i], writes=["vst%d" % s])
                P.dma("sync", gb[s][:, i, :], G_d[c0 + i], reads=["Gd"], writes=["gb%d" % s])
            k.tt("gpsimd", ub[s][:].rearrange("p g (j n) -> p g j n", j=8), ust[s][:].rearrange("p g (j n) -> p g j n", j=8), g2b4, ALU.mult, ["ust%d" % s, "pv"], ["ub%d" % s])
            k.cp("gpsimd", vb[s][:], vst[s][:], ["vst%d" % s], ["vb%d" % s])

        def stage1(n):
            grp, q = steps[n]
            s = grp % 2
            pr = quads[q]
            t0 = pr[0] * 128
            nt = len(pr) * 128
            a_ = amq[n % 2]
            for i in range(GS):
                sb_ = cnt["S"] % 2
                cnt["S"] += 1
                for j in range(8):
                    k.mm(pS[sb_][:, 0:nt], ub[s][:, i, j * 128:(j + 1) * 128], hn2T[:, j, t0:t0 + nt], j == 0, j == 7, ["ub%d" % s, "hn2T"], ["q%d" % sb_])
                k.act(gl[sb_][:, 0:nt], pS[sb_][:, 0:nt], AF.Gelu, ["q%d" % sb_], ["gl%d" % sb_])
                k.tt("gpsimd", a_[:, i, 0:nt], gl[sb_][:, 0:nt], gb[s][:, i, t0:t0 + nt], ALU.mult, ["gl%d" % sb_, "gb%d" % s], ["amq%d" % (n % 2)])

        def stage2(n):
            grp, q = steps[n]
            s = grp % 2
            pr = quads[q]
            a_ = amq[n % 2]
            for tt, ti in enumerate(pr):
                st_ = cnt["set"] % 3
                cnt["set"] += 1
                for hf in range(2):
                    bk = st_ * 2 + hf
                    for i in range(GS):
                        k.mm(pA[bk][:], a_[:, i, tt * 128:(tt + 1) * 128], vb[s][:, i, hf * 512:(hf + 1) * 512], i == 0, i == GS - 1, ["amq%d" % (n % 2), "vb%d" % s], ["q%d" % (2 + bk)])
                for hf in range(2):
                    bk = st_ * 2 + hf
                    k.tt("vector", acc[:, ti, hf * 512:(hf + 1) * 512], acc[:, ti, hf * 512:(hf + 1) * 512], pA[bk][:], ALU.add, ["acc%d" % ti, "q%d" % (2 + bk)], ["acc%d" % ti])

        loads(0)
        if NGRP > 1:
            loads(1)
        stage1(0)
        for n in range(len(steps)):
            if n + 1 < len(steps):
                stage1(n + 1)
            stage2(n)
            grp, q = steps[n]
            if q == len(quads) - 1 and grp + 2 < NGRP:
                loads(grp + 2)
        evs = []
        for ti in PTILES:
            ak = "acc%d" % ti
            k.memset("vector", sm[:, 0:1], 0.0, ["fsm0"])
            P.op("scalar", lambda e, ti=ti: e.activation(out=junk[:], in_=acc[:, ti, :], func=AF.Square, scale=1.0 / 32.0, accum_out=sm[:, 0:1]), [ak, "fsm0"], ["fjunk", "fsm0"])
            k.act(sm[:, 1:2], sm[:, 0:1], AF.Ln, ["fsm0", "eps6"], ["fsm1"], bias=eps6[:])
            k.act(sm[:, 1:2], sm[:, 1:2], AF.Exp, ["fsm1"], ["fsm1"], scale=-0.5)
            k.stt(acc[:, ti, :], acc[:, ti, :], sm[:, 1:2], fgb[:], ALU.mult, ALU.mult, [ak, "fsm1", "fgb"], [ak])
            evs.append(P.dma("sync", out_d[ti * 128:(ti + 1) * 128, :], acc[:, ti, :], reads=[ak]))
        P.final_wait("sync", evs)
        with nc.Block() as block:
            P.emit(block)


def _host_inputs(inp):
    f = lambda a: np.ascontiguousarray(np.asarray(a, dtype=np.float32))
    x = f(inp["x"])
    w_in = f(inp["w_in"])[0]
    colidx = np.concatenate([np.asarray(c, dtype=np.int64) for _, c in PIECES])
    w_perm = np.ascontiguousarray(w_in[:, colidx])
    mu = f(inp["rw_mu"])[0]
    mu_cols = mu[colidx[:NMU] - RW0]
    mu_perm = np.ascontiguousarray(np.broadcast_to(mu_cols[None, :], (128, NMU)))
    pvh = np.zeros((128, NPV), np.float32)
    ch = lambda v, n: np.asarray(v, np.float32).reshape(n, 128).T
    pvh[:, PV["g1"]:PV["g1"] + 8] = ch(inp["norm1_g"][0], 8)
    for nm, key in [("w0", "rw_w0"), ("a0", "rw_a0"), ("k_k", "rw_k_k"), ("k_a", "rw_k_a"), ("gn_w", "rw_gn_w"), ("gn_b", "rw_gn_b"),
                    ("cq_b", "ml_conv_q_b"), ("ck_b", "ml_conv_k_b")]:
        pvh[:, PV[nm]:PV[nm] + 4] = ch(inp[key][0], 4)
    pvh[:, PV["r_k"]:PV["r_k"] + 4] = ch(np.asarray(inp["rw_r_k"][0]).reshape(512), 4)
    for nm, key in [("cq_w", "ml_conv_q_w"), ("ck_w", "ml_conv_k_w")]:
        w = np.asarray(inp[key][0], np.float32)
        for tap in range(4):
            pvh[:, PV[nm] + tap * 4:PV[nm] + tap * 4 + 4] = ch(w[tap], 4)
    pvh[:, PV["gate_b"]:PV["gate_b"] + 16] = ch(inp["gate_b"][0], 16)
    pvh[:, PV["g2"]:PV["g2"] + 8] = ch(inp["norm2_g"][0], 8)
    pvh[:, PV["b_i"]:PV["b_i"] + 4] = np.asarray(inp["ml_b_i"][0], np.float32)[None, :]
    pvh[:, PV["b_f"]:PV["b_f"] + 4] = np.asarray(inp["ml_b_f"][0], np.float32)[None, :]
    lora = np.concatenate([f(inp["rw_w_up"])[0], f(inp["rw_a_up"])[0]], axis=0)
    sk = f(inp["peer_sub_keys"])[0]
    skT = np.ascontiguousarray(sk.reshape(16, 128, 128).transpose(2, 0, 1).reshape(128, 16 * 128))
    U = f(inp["peer_u"])[0]
    uT = np.ascontiguousarray(U.reshape(128, 128, 8, 128).transpose(1, 3, 2, 0).reshape(128, 128, 1024))
    V = f(inp["peer_v"])[0]
    vp = np.ascontiguousarray(V.reshape(128, 128, 1024).transpose(1, 0, 2))
    common = {
        "w_perm": w_perm, "mu_perm": mu_perm, "lora_w": np.ascontiguousarray(lora), "g_up": f(inp["rw_g_up"])[0],
        "p_rw": f(inp["p_rw"])[0], "p_ml": f(inp["p_ml"])[0], "w_out": f(inp["w_out"])[0],
        "ml_norm_w": f(inp["ml_norm_w"])[0][None, :], "peer_w_q": f(inp["peer_w_q"])[0], "skT": skT, "uT": uT, "vp": vp,
        "final_g": f(inp["final_g"])[None, :],
    }
    maps = []
    for c in range(8):
        b, half = c // 2, c % 2
        xs = np.zeros((4096, 1024), np.float32)
        if half == 0:
            xs[2048:] = x[b, :2048]
        else:
            xs[:] = x[b]
        pvc = pvh.copy()
        pvc[:, PV["flag"]] = float(half)
        m = dict(common)
        m["xs"] = xs
        m["pv"] = pvc
        maps.append(m)
    return maps


def kernel(**inputs):
    maps = _host_inputs(inputs)
    nc = build_nc()
    res = run_bass_kernel_spmd(nc, maps, core_ids=list(range(8)))
    out = np.zeros((4, 4096, 1024), np.float32)
    for c in range(8):
        b, half = c // 2, c % 2
        out[b, half * 2048:(half + 1) * 2048] = res.results[c]["out"]
    return out
```
